# Optimizing a Trainium2 kernel written in Bass

```python
import jax, jax.numpy as jnp
from jax import lax
import numpy as np

D_MODEL = 2048
BATCH = 8
SEQ = 4096
DEPTH = 2

GRID_W = 64
CTX_LEN = 256
N_MIXERS = 2
POOL_WINDOWS = (2, 4, 8, 16)
POOL_GROUPS = 4
POOL_GROUP_DIM = D_MODEL // POOL_GROUPS
HGRN_HEADS = 16
HGRN_EXPAND = D_MODEL // HGRN_HEADS
HGRN_HEAD_V = D_MODEL // HGRN_HEADS
HGRN_FDIM = HGRN_HEADS * HGRN_EXPAND
CHUNK = 32
N_GROUPS = 4
EXPERTS_PER_GROUP = 8
N_EXPERTS = N_GROUPS * EXPERTS_PER_GROUP
TOP_K = 2
D_EXPERT = 1024
MOE_BLOCK = 128
EPS = 1e-6

kernel_name = "hybrid_pool_hgrn2_hmoe_prefix_dit"


def rms_norm(x, g):
    xf = x.astype(jnp.float32)
    y = xf * lax.rsqrt(jnp.mean(xf * xf, axis=-1, keepdims=True) + EPS)
    return (y * g.astype(jnp.float32)).astype(x.dtype)


def modulate(h, shift, scale):
    return h * (1 + scale) + shift


def adaln(cond, w, b):
    m = (jax.nn.silu(cond) @ w + b)[:, None, :]
    return jnp.split(m, 6, axis=-1)


def centred_window_mean(x, k, axis):
    xf = x.astype(jnp.float32)
    L = x.shape[axis]
    cs = jnp.cumsum(xf, axis=axis)
    pad = [(0, 0)] * x.ndim
    pad[axis] = (1, 0)
    cs = jnp.pad(cs, pad)
    pos = np.arange(L)
    lo = np.clip(pos - k // 2, 0, L - 1)
    hi = np.clip(pos + (k - k // 2 - 1), 0, L - 1)
    s = jnp.take(cs, hi + 1, axis=axis) - jnp.take(cs, lo, axis=axis)
    shape = [1] * x.ndim
    shape[axis] = L
    cnt = (hi - lo + 1).astype(np.float32).reshape(shape)
    return s / cnt


def pool_mixer(h, w_pool, scale, on_grid):
    B, T, _ = h.shape
    outs = []
    for j, k in enumerate(POOL_WINDOWS):
        hg = h[..., j * POOL_GROUP_DIM:(j + 1) * POOL_GROUP_DIM]
        if on_grid:
            rows = T // GRID_W
            g = hg.reshape(B, rows, GRID_W, POOL_GROUP_DIM)
            m = centred_window_mean(centred_window_mean(g, k, 1), k, 2).reshape(B, T, POOL_GROUP_DIM)
        else:
            m = centred_window_mean(hg, k, 1)
        d = (m - hg.astype(jnp.float32)).astype(h.dtype)
        outs.append(d @ w_pool[j])
    return jnp.concatenate(outs, axis=-1) * scale


def forget_gate(z, lb):
    z = z.astype(jnp.float32)
    logf = jnp.logaddexp(jnp.log(lb), jnp.log1p(-lb) + jax.nn.log_sigmoid(z))
    return logf, -jnp.expm1(logf)


def gla_chunked(q, k, v, logf, s0):
    B, L, H, _ = q.shape
    n = L // CHUNK

    def to_chunks(a):
        return a.reshape(B, n, CHUNK, H, a.shape[-1]).transpose(1, 0, 3, 2, 4)

    causal = jnp.tril(jnp.ones((CHUNK, CHUNK), dtype=bool))[:, :, None]

    def step(S, inp):
        qc, kc, vc, lc = inp
        b = jnp.cumsum(lc, axis=2)
        diff = b[:, :, :, None, :] - b[:, :, None, :, :]
        decay = jnp.exp(jnp.where(causal, diff, -jnp.inf))
        A = jnp.einsum('bhtd,bhsd,bhtsd->bhts', qc, kc, decay)
        o = jnp.einsum('bhts,bhsv->bhtv', A, vc) + jnp.einsum('bhtd,bhdv->bhtv', qc * jnp.exp(b), S)
        bl = b[:, :, -1:, :]
        S = jnp.exp(bl[:, :, 0, :])[..., None] * S + jnp.einsum('bhsd,bhsv->bhdv', kc * jnp.exp(bl - b), vc)
        return S, o

    S, o = lax.scan(step, s0, (to_chunks(q), to_chunks(k), to_chunks(v), to_chunks(logf)))
    o = o.transpose(1, 0, 3, 2, 4).reshape(B, L, H, v.shape[-1])
    return o, S


def gla_final_state(k, v, logf):
    b = jnp.cumsum(logf, axis=1)
    return jnp.einsum('blhd,blhv->bhdv', k * jnp.exp(b[:, -1:] - b), v)


def hgrn2_mixer(hx, hc, w_in, norm_g, w_out, lb, ctx_out):
    Dm = D_MODEL

    def heads(a):
        return a.reshape(a.shape[0], a.shape[1], HGRN_HEADS, -1)

    def flip(a):
        return jnp.flip(a, axis=1)

    def read(o, g):
        on = o * lax.rsqrt(jnp.mean(o * o, axis=-1, keepdims=True) + EPS) * norm_g.astype(jnp.float32)
        y = on * jax.nn.silu(heads(g.astype(jnp.float32)))
        return y.reshape(o.shape[0], o.shape[1], Dm).astype(hx.dtype) @ w_out

    if ctx_out:
        qc, gc, zfc, zbc, ic = jnp.split(hc @ w_in, 5, axis=-1)
    else:
        zfc, zbc, ic = jnp.split(hc @ w_in[:, 2 * Dm:], 3, axis=-1)
    lfc, kfc = forget_gate(zfc, lb[0])
    lbc, kbc = forget_gate(zbc, lb[1])
    lfc, kfc, lbc, kbc = heads(lfc), heads(kfc), heads(lbc), heads(kbc)
    vc = heads(ic.astype(jnp.float32))
    if ctx_out:
        qch = heads(jax.nn.silu(qc.astype(jnp.float32)))
        s0 = jnp.zeros((hc.shape[0], HGRN_HEADS, HGRN_EXPAND, HGRN_HEAD_V), jnp.float32)
        oc_f, s_f = gla_chunked(qch, kfc, vc, lfc, s0)
        oc_b, s_b = gla_chunked(flip(qch), flip(kbc), flip(vc), flip(lbc), s0)
        yc = read(oc_f + flip(oc_b), gc)
    else:
        s_f = gla_final_state(kfc, vc, lfc)
        s_b = gla_final_state(flip(kbc), flip(vc), flip(lbc))
        yc = None

    qx, gx, zfx, zbx, ix = jnp.split(hx @ w_in, 5, axis=-1)
    lfx, kfx = forget_gate(zfx, lb[0])
    lbx, kbx = forget_gate(zbx, lb[1])
    qxh = heads(jax.nn.silu(qx.astype(jnp.float32)))
    vx = heads(ix.astype(jnp.float32))
    ox_f, _ = gla_chunked(qxh, heads(kfx), vx, heads(lfx), s_f)
    ox_b, _ = gla_chunked(flip(qxh), flip(heads(kbx)), flip(vx), flip(heads(lbx)), s_b)
    yx = read(ox_f + flip(ox_b), gx)
    return yx, yc


def moe_ffn(h, w_grp, b_grp, w_exp, b_exp, w_gu, w_down):
    N, Dm = h.shape
    hf = h.astype(jnp.float32)
    p_grp = jax.nn.softmax(hf @ w_grp.astype(jnp.float32) + b_grp.astype(jnp.float32), axis=-1)
    g_idx = jnp.argmax(p_grp, axis=-1)
    p_g = jnp.max(p_grp, axis=-1)
    le = (hf @ w_exp.astype(jnp.float32) + b_exp.astype(jnp.float32)).reshape(N, N_GROUPS, EXPERTS_PER_GROUP)
    le = jnp.take_along_axis(le, g_idx[:, None, None], axis=1)[:, 0]
    vals, e_in = lax.top_k(jax.nn.softmax(le, axis=-1), TOP_K)
    wts = p_g[:, None] * vals / jnp.sum(vals, axis=-1, keepdims=True)
    eid = g_idx[:, None] * EXPERTS_PER_GROUP + e_in

    S = N * TOP_K
    e_flat = eid.reshape(-1)
    tok = jnp.repeat(jnp.arange(N), TOP_K)
    order = jnp.argsort(e_flat)
    e_s, tok_s, w_s = e_flat[order], tok[order], wts.reshape(-1)[order]
    counts = jnp.bincount(e_flat, length=N_EXPERTS)
    starts = jnp.cumsum(counts) - counts
    padded = ((counts + MOE_BLOCK - 1) // MOE_BLOCK) * MOE_BLOCK
    pends = jnp.cumsum(padded)
    pstarts = pends - padded
    dest = pstarts[e_s] + (jnp.arange(S) - starts[e_s])
    n_blocks = -(-(S + N_EXPERTS * (MOE_BLOCK - 1)) // MOE_BLOCK)
    P = n_blocks * MOE_BLOCK
    xs = jnp.zeros((P, Dm), h.dtype).at[dest].set(h[tok_s])
    block_e = jnp.clip(jnp.searchsorted(pends, jnp.arange(n_blocks) * MOE_BLOCK, side='right'), 0, N_EXPERTS - 1)

    def block_fn(args):
        xb, e = args
        a, b = jnp.split(xb @ w_gu[e], 2, axis=-1)
        return (jax.nn.silu(a) * b) @ w_down[e]

    ys = lax.map(block_fn, (xs.reshape(n_blocks, MOE_BLOCK, Dm), block_e)).reshape(P, Dm)
    out = jnp.zeros((N, Dm), jnp.float32).at[tok_s].add(w_s[:, None] * ys[dest].astype(jnp.float32))
    return out.astype(h.dtype)


def setup_inputs(seed: int = 0) -> dict:
    key = jax.random.key(seed)
    ks = jax.random.split(key, 24)
    n_pool = (DEPTH + N_MIXERS - 1) // N_MIXERS
    n_hgrn = DEPTH // N_MIXERS
    D = D_MODEL
    nrm = jax.random.normal
    f32 = jnp.float32
    return {
        "x": nrm(ks[0], (BATCH, SEQ, D), f32),
        "c": nrm(ks[1], (BATCH, D), f32),
        "ctx": nrm(ks[2], (BATCH, CTX_LEN, D), f32),
        "c_ctx": nrm(ks[3], (D,), f32),
        "norm_mix": 1.0 + 0.05 * nrm(ks[4], (DEPTH, D), f32),
        "norm_ffn": 1.0 + 0.05 * nrm(ks[5], (DEPTH, D), f32),
        "w_ada": 0.5 * D ** -0.5 * nrm(ks[6], (DEPTH, D, 6 * D), f32),
        "b_ada": 0.01 * nrm(ks[7], (DEPTH, 6 * D), f32),
        "pool_w": POOL_GROUP_DIM ** -0.5 * nrm(ks[8], (n_pool, POOL_GROUPS, POOL_GROUP_DIM, POOL_GROUP_DIM), f32),
        "pool_scale": 1.0 + 0.1 * nrm(ks[9], (n_pool, D), f32),
        "hgrn_w_in": D ** -0.5 * nrm(ks[10], (n_hgrn, D, 5 * D), f32),
        "hgrn_norm": 1.0 + 0.05 * nrm(ks[11], (n_hgrn, HGRN_HEAD_V), f32),
        "hgrn_w_out": D ** -0.5 * nrm(ks[12], (n_hgrn, D, D), f32),
        "hgrn_lb_logits": nrm(ks[13], (DEPTH, 2, HGRN_FDIM), f32),
        "router_w_group": D ** -0.5 * nrm(ks[14], (DEPTH, D, N_GROUPS), f32),
        "router_b_group": 0.01 * nrm(ks[15], (DEPTH, N_GROUPS), f32),
        "router_w_expert": D ** -0.5 * nrm(ks[16], (DEPTH, D, N_EXPERTS), f32),
        "router_b_expert": 0.01 * nrm(ks[17], (DEPTH, N_EXPERTS), f32),
        "moe_w_gate_up": D ** -0.5 * nrm(ks[18], (DEPTH, N_EXPERTS, D, 2 * D_EXPERT), f32),
        "moe_w_down": D_EXPERT ** -0.5 * nrm(ks[19], (DEPTH, N_EXPERTS, D_EXPERT, D), f32),
        "norm_final": 1.0 + 0.05 * nrm(ks[20], (D,), f32),
    }


def reference(x, c, ctx, c_ctx, norm_mix, norm_ffn, w_ada, b_ada, pool_w, pool_scale, hgrn_w_in, hgrn_norm,
              hgrn_w_out, hgrn_lb_logits, router_w_group, router_b_group, router_w_expert, router_b_expert,
              moe_w_gate_up, moe_w_down, norm_final):
    B, T, D = x.shape
    p_lb = jax.nn.softmax(hgrn_lb_logits.astype(jnp.float32), axis=0)
    lb_all = jnp.cumsum(p_lb, axis=0) - p_lb[0]
    for i in range(DEPTH):
        ctx_needed = i < DEPTH - 1
        j = i // N_MIXERS
        sh_m, sc_m, gt_m, sh_f, sc_f, gt_f = adaln(c, w_ada[i], b_ada[i])
        csh_m, csc_m, cgt_m, csh_f, csc_f, cgt_f = adaln(c_ctx[None, :], w_ada[i], b_ada[i])
        hx = modulate(rms_norm(x, norm_mix[i]), sh_m, sc_m)
        if i % N_MIXERS == 0:
            yx = pool_mixer(hx, pool_w[j], pool_scale[j], on_grid=True)
            if ctx_needed:
                hc = modulate(rms_norm(ctx, norm_mix[i]), csh_m, csc_m)
                yc = pool_mixer(hc, pool_w[j], pool_scale[j], on_grid=False)
        else:
            hc = modulate(rms_norm(ctx, norm_mix[i]), csh_m, csc_m)
            yx, yc = hgrn2_mixer(hx, hc, hgrn_w_in[j], hgrn_norm[j], hgrn_w_out[j], lb_all[i], ctx_needed)
        x = x + gt_m * yx
        if ctx_needed:
            ctx = ctx + cgt_m * yc
        fx = modulate(rms_norm(x, norm_ffn[i]), sh_f, sc_f).reshape(B * T, D)
        moe_args = (router_w_group[i], router_b_group[i], router_w_expert[i], router_b_expert[i],
                    moe_w_gate_up[i], moe_w_down[i])
        if ctx_needed:
            L = ctx.shape[1]
            fc = modulate(rms_norm(ctx, norm_ffn[i]), csh_f, csc_f).reshape(B * L, D)
            y_all = moe_ffn(jnp.concatenate([fx, fc], axis=0), *moe_args)
            x = x + gt_f * y_all[:B * T].reshape(B, T, D)
            ctx = ctx + cgt_f * y_all[B * T:].reshape(B, L, D)
        else:
            x = x + gt_f * moe_ffn(fx, *moe_args).reshape(B, T, D)
    return rms_norm(x, norm_final)
```

```python
import numpy as np
from contextlib import ExitStack
import concourse.bass as bass
import concourse.mybir as mybir
from concourse.bass_utils import run_bass_kernel_spmd

F32 = mybir.dt.float32
BF16 = mybir.dt.bfloat16
I32 = mybir.dt.int32
ALU = mybir.AluOpType
AF = mybir.ActivationFunctionType
AX = mybir.AxisListType
EPS = 1e-6

FULL_CFG = dict(D=2048, L=4096, GW=64, LC=256, NH=16, NG=4, EG=8, DE=1024, CAP=512, NS=8,
                WINS=(2, 4, 8, 16), NCORES=8)


class Buf:
    def __init__(self, t, name):
        self.t = t
        self.name = name
        self.w = None
        self.r = []
        self.dsem = None
        self.dcnt = 0

    def __getitem__(self, k):
        return self.t[k]

    def sub(self, key, name=None):
        return Buf(self.t[key], name or self.name + "_s")


class Eng:
    def __init__(self, name, sem):
        self.name = name
        self.sem = sem
        self.cnt = 0
        self.seen = {}
        self.prog = []


class KB:
    def __init__(self, nc, ctx, nsem=84):
        self.nc = nc
        self.gctx = ctx
        self.ctx = ctx
        self.E = {}
        for name in ("pe", "act", "dve", "pool", "sp"):
            s = ctx.enter_context(nc.semaphore("es_" + name))
            self.E[name] = Eng(name, s)
        self.pool_sems = [ctx.enter_context(nc.semaphore("ds%d" % i)) for i in range(nsem)]
        self.semval = {id(s): 0 for s in self.pool_sems}
        self.free_sems = list(self.pool_sems)
        self.phase_bufs = []
        self.n_ins = 0

    def sb(self, name, shape, dtype):
        self.uid = getattr(self, "uid", 0) + 1
        name = "s%d_%s" % (self.uid, name)
        t = self.ctx.enter_context(self.nc.sbuf_tensor(name, list(shape), dtype))
        return Buf(t, name)

    def ps(self, name, shape, dtype=F32):
        self.uid = getattr(self, "uid", 0) + 1
        name = "p%d_%s" % (self.uid, name)
        t = self.ctx.enter_context(self.nc.psum_tensor(name, list(shape), dtype))
        return Buf(t, name)

    def dram(self, name, shape, dtype, kind="Internal"):
        t = self.nc.dram_tensor(name, list(shape), dtype, kind=kind)
        return Buf(t.ap(), name)

    def _dsem(self, b):
        if b.dsem is None:
            b.dsem = self.free_sems.pop()
            b.dcnt = self.semval[id(b.dsem)]
            self.phase_bufs.append(b)
        return b.dsem

    def _filter(self, eng, deps, raw_same):
        out = {}
        for (sem, val, e) in deps:
            if e == eng.name and not raw_same:
                continue
            k = id(sem)
            if k not in out or out[k][1] < val:
                out[k] = (sem, val)
        res = []
        for k, (sem, val) in out.items():
            if eng.seen.get(k, 0) >= val:
                continue
            eng.seen[k] = val
            res.append((sem, val))
        return res

    def op(self, en, fn, reads=(), writes=(), raw_same=None):
        eng = self.E[en]
        if raw_same is None:
            raw_same = en != "pe"
        deps = []
        for b in reads:
            if b.w is not None:
                deps.append(b.w)
        for b in writes:
            if b.w is not None:
                deps.append(b.w)
            for r in b.r:
                if r[2] != en:
                    deps.append(r)
        waits = self._filter(eng, deps, raw_same)
        eng.cnt += 1
        rec = (eng.sem, eng.cnt, en)
        eng.prog.append((waits, fn, (eng.sem, 1)))
        for b in reads:
            b.r.append(rec)
        for b in writes:
            b.w = rec
            b.r = []
        self.n_ins += 1
        return rec

    def dma(self, qn, fn, reads=(), writes=(), fill=False):
        eng = self.E[qn]
        deps = []
        for b in reads:
            if b.w is not None:
                deps.append(b.w)
        for b in writes:
            if b.w is not None and not (fill and b.w[2] == "dma"):
                deps.append(b.w)
            deps.extend(b.r)
        waits = self._filter(eng, deps, True)
        tgt = writes[0]
        sem = self._dsem(tgt)
        tgt.dcnt += 16
        self.semval[id(sem)] = tgt.dcnt
        rec = (sem, tgt.dcnt, "dma")
        eng.prog.append((waits, fn, (sem, 16)))
        for b in reads:
            b.r.append(rec)
        for b in writes:
            b.w = rec
            if not fill:
                b.r = []
        self.n_ins += 1
        return rec

    def pool_reg(self, value):
        if getattr(self, "_preg", None) is None:
            self._preg = self.nc.alloc_register(mybir.EngineType.Pool, "bcreg")
        reg = self._preg
        self.E["pool"].prog.append(([], lambda h: h.reg_mov(reg, value), None))
        return reg

    def barrier(self):
        targets = [(e.sem, e.cnt) for e in self.E.values() if e.cnt > 0]
        for b in self.phase_bufs:
            targets.append((b.dsem, b.dcnt))
        for eng in self.E.values():
            waits = []
            for (sem, val) in targets:
                if sem is eng.sem:
                    continue
                if eng.seen.get(id(sem), 0) >= val:
                    continue
                eng.seen[id(sem)] = val
                waits.append((sem, val))
            eng.prog.append((waits, None, None))

    def end_phase(self):
        self.barrier()
        self.emit()
        for b in self.phase_bufs:
            self.free_sems.append(b.dsem)
            b.dsem = None
        self.phase_bufs = []

    def emit(self):
        nc = self.nc
        with nc.Block() as block:
            def run(eng):
                def body(h):
                    for waits, fn, inc in eng.prog:
                        for (sem, val) in waits:
                            h.wait_ge(sem, val)
                        if fn is not None:
                            ins = fn(h)
                            if inc is not None:
                                ins.then_inc(inc[0], inc[1])
                    eng.prog = []
                return body
            block.tensor(run(self.E["pe"]))
            block.scalar(run(self.E["act"]))
            block.vector(run(self.E["dve"]))
            block.gpsimd(run(self.E["pool"]))
            block.sync(run(self.E["sp"]))

    @staticmethod
    def _s(x):
        return x[1] if isinstance(x, tuple) else x

    @staticmethod
    def _bufs(*xs):
        return [x[0] for x in xs if isinstance(x, tuple)]

    def mm(self, out, lhsT, rhs, start=True, stop=True, tr=False):
        self.op("pe", lambda h: h.matmul(out[1], lhsT=lhsT[1], rhs=rhs[1], start=start, stop=stop,
                                          is_transpose=(True if tr else None)),
                reads=[lhsT[0], rhs[0]], writes=[out[0]])

    def tt(self, en, out, in0, in1, op):
        self.op(en, lambda h: h.tensor_tensor(out=out[1], in0=in0[1], in1=in1[1], op=op),
                reads=[in0[0], in1[0]], writes=[out[0]])

    def ts(self, en, out, in0, s1, s2=None, op0=ALU.mult, op1=None, accum=None):
        kw = {}
        if op1 is not None:
            kw["op1"] = op1
        if accum is not None:
            kw["accum_out"] = accum[1]
        self.op(en, lambda h: h.tensor_scalar(out=out[1], in0=in0[1], scalar1=self._s(s1), scalar2=self._s(s2),
                                               op0=op0, **kw),
                reads=[in0[0]] + self._bufs(s1, s2), writes=[out[0]] + self._bufs(accum))

    def stt(self, en, out, in0, scalar, in1, op0, op1):
        self.op(en, lambda h: h.scalar_tensor_tensor(out=out[1], in0=in0[1], scalar=self._s(scalar), in1=in1[1],
                                                      op0=op0, op1=op1),
                reads=[in0[0], in1[0]] + self._bufs(scalar), writes=[out[0]])

    def act(self, out, in_, func, bias=None, scale=None, accum=None):
        kw = {}
        if bias is not None:
            kw["bias"] = self._s(bias)
        if scale is not None:
            kw["scale"] = self._s(scale)
        if accum is not None:
            kw["accum_out"] = accum[1]
        self.op("act", lambda h: h.activation(out=out[1], in_=in_[1], func=func, **kw),
                reads=[in_[0]] + self._bufs(bias, scale), writes=[out[0]] + self._bufs(accum))

    def cp(self, en, out, in_):
        if en == "act":
            self.op(en, lambda h: h.copy(out=out[1], in_=in_[1]), reads=[in_[0]], writes=[out[0]])
        else:
            self.op(en, lambda h: h.tensor_copy(out=out[1], in_=in_[1]), reads=[in_[0]], writes=[out[0]])

    def red(self, en, out, in_, op, axis=AX.X):
        self.op(en, lambda h: h.tensor_reduce(out=out[1], in_=in_[1], axis=axis, op=op),
                reads=[in_[0]], writes=[out[0]])

    def memset(self, en, out, val):
        self.op(en, lambda h: h.memset(out[1], val), reads=[], writes=[out[0]])

    def ld(self, qn, out, in_, fill=False):
        self.dma(qn, lambda h: h.dma_start(out=out[1], in_=in_[1]), reads=[in_[0]], writes=[out[0]], fill=fill)


def V(b, key=None):
    return (b, b.t[key] if key is not None else b.t[:])


def _win(k):
    return k // 2, k - k // 2 - 1


def pool_consts_grid(cfg):
    GW, L, WINS = cfg["GW"], cfg["L"], cfg["WINS"]
    RPT = 128 // GW
    NT = L // 128
    rows = L // GW
    offs = []
    blocks = []
    bidx = {}
    cls_rows = []
    cls_of = {}
    diag_idx = {}
    rs = np.arange(128) // GW
    cs = np.arange(128) % GW
    for j, k in enumerate(WINS):
        lo, hi = _win(k)
        dlo = (-lo) // RPT
        dhi = (RPT - 1 + hi) // RPT
        offs.append(list(range(dlo, dhi + 1)))
        for d in range(dlo, dhi + 1):
            rd = (RPT * d + rs[:, None]) - rs[None, :]
            cd = cs[:, None] - cs[None, :]
            W = ((rd >= -lo) & (rd <= hi) & (cd >= -lo) & (cd <= hi)).astype(np.float32)
            if d != 0:
                bidx[(j, d)] = len(blocks)
                blocks.append(W)
            else:
                W0 = W
        for i in range(NT):
            r = RPT * i + rs
            cr = np.minimum(r + hi, rows - 1) - np.maximum(r - lo, 0) + 1
            cc = np.minimum(cs + hi, GW - 1) - np.maximum(cs - lo, 0) + 1
            cnt = (cr * cc).astype(np.float32)
            key = (j, tuple(cnt.tolist()))
            if key not in diag_idx:
                diag_idx[key] = (len(blocks), len(cls_rows))
                blocks.append(W0 - np.diag(cnt))
                cls_rows.append(1.0 / cnt)
            cls_of[(j, i)] = diag_idx[key]
    B = np.stack(blocks, 1).astype(np.float32)
    RC = np.stack(cls_rows, 0).astype(np.float32)
    RC = np.broadcast_to(RC[None], (128,) + RC.shape).copy()
    return offs, bidx, cls_of, B, RC


def pool_consts_seq(cfg):
    LC, WINS = cfg["LC"], cfg["WINS"]
    NCT = LC // 128
    pos = np.arange(LC)
    Bs = np.zeros((128, len(WINS), NCT, NCT, 128), np.float32)
    RC = np.zeros((len(WINS), NCT, 128), np.float32)
    for j, k in enumerate(WINS):
        lo, hi = _win(k)
        d = pos[:, None] - pos[None, :]
        W = ((d >= -lo) & (d <= hi)).astype(np.float32)
        cnt = (np.minimum(pos + hi, LC - 1) - np.maximum(pos - lo, 0) + 1).astype(np.float32)
        W = W - np.diag(cnt)
        for a in range(NCT):
            for b in range(NCT):
                Bs[:, j, a, b, :] = W[a * 128:(a + 1) * 128, b * 128:(b + 1) * 128]
        RC[j] = (1.0 / cnt).reshape(NCT, 128)
    RC = np.broadcast_to(RC[None], (128,) + RC.shape).copy()
    return Bs, RC


class Prog:
    def __init__(self, cfg, debug=False, stop_after=None):
        self.cfg = cfg
        self.debug = debug
        self.stop_after = stop_after
        c = cfg
        self.D, self.L, self.LC = c["D"], c["L"], c["LC"]
        self.KD = self.D // 128
        self.NT = self.L // 128
        self.NCT = self.LC // 128
        self.NE = c["NG"] * c["EG"]
        self.NR = c["NG"] + self.NE
        self.offs, self.bidx, self.cls_of, self.Bgrid, self.RCgrid = pool_consts_grid(cfg)
        self.Bseq, self.RCseq = pool_consts_seq(cfg)

    def build(self):
        c = self.cfg
        D, L, LC, KD, NE = self.D, self.L, self.LC, self.KD, self.NE
        nc = bass.Bass("TRN2", target_bir_lowering=False)
        self.nc = nc
        with ExitStack() as gctx:
            kb = KB(nc, gctx)
            self.kb = kb
            I = {}

            def inp(name, shape, dt=F32):
                I[name] = kb.dram(name, shape, dt, kind="ExternalInput")
            inp("x", [L, D]); inp("ctx", [LC, D]); inp("cvec", [128, KD, 2])
            inp("w_ada", [2, D, 6 * D]); inp("b_ada", [2, 2, 6 * D])
            inp("gains", [6, D])
            inp("pool_w", [4, D // 4, D // 4])
            inp("bgrid", list(self.Bgrid.shape)); inp("rcgrid", list(self.RCgrid.shape))
            inp("bseq", list(self.Bseq.shape)); inp("rcseq", list(self.RCseq.shape))
            inp("ident", [128, 128])
            inp("wr", [2, D, self.NR]); inp("br", [2, 128, self.NR])
            inp("ecap", [128, NE]); inp("tri", [128, 128]); inp("pcol", [128, 1])
            inp("w_gu", [2, NE, D, 2 * c["DE"]]); inp("w_dn", [2, NE, c["DE"], D])
            inp("w_in", [D, 5 * D]); inp("w_out", [D, D]); inp("lbl", [128, 2, 2, c["NH"]]); inp("hnorm", [128, 1])
            self.SEG = min(1024, L)
            inp("mask128", [128, self.SEG]); inp("mask32", [128, self.SEG]); inp("trimask", [128, 2, 128])
            self.I = I
            okind = "ExternalOutput" if self.debug else "Internal"
            S = {}
            S["MOD0"] = kb.dram("MOD0", [2, 6 * D], F32, kind=okind)
            S["MOD1"] = kb.dram("MOD1", [2, 6 * D], F32, kind=okind)
            S["X1"] = kb.dram("X1", [L, D], F32, kind=okind)
            S["C1"] = kb.dram("C1", [LC, D], F32, kind=okind)
            S["X2"] = kb.dram("X2", [L, D], F32, kind=okind)
            S["C2"] = kb.dram("C2", [LC, D], F32, kind=okind)
            S["X3"] = kb.dram("X3", [L, D], F32, kind=okind)
            NROWS = NE * c["CAP"] + c["NS"] * 128
            S["XS"] = kb.dram("XS", [NROWS, D], BF16)
            S["YS"] = kb.dram("YS", [NROWS, D], F32)
            S["FB"] = kb.dram("FB", [L + LC, D], BF16)
            S["HXT"] = kb.dram("HXT", [D, L], BF16)
            S["HCT"] = kb.dram("HCT", [D, LC], BF16)
            S["YT"] = kb.dram("YT", [D, L], BF16)
            S["OUT"] = kb.dram("out", [L, D], F32, kind="ExternalOutput")
            self.S = S

            stages = [("ada", self.phase_ada),
                      ("mix0", self.phase_pool),
                      ("moe0", lambda: self.phase_moe(0)),
                      ("hgrn", self.phase_hgrn),
                      ("moe1", lambda: self.phase_moe(1))]
            for name, fn in stages:
                with ExitStack() as pctx:
                    kb.ctx = pctx
                    fn()
                    kb.end_phase()
                kb.ctx = gctx
                if self.stop_after == name:
                    break
        return nc

    def load_bcast(self, kb, dst, src_row_ap, src_buf, qn="sp"):
        kb.dma(qn, lambda h: h.dma_start(out=dst[1], in_=src_row_ap.partition_broadcast(128)),
               reads=[src_buf], writes=[dst[0]])

    def rstd_of(self, kb, rstd, ssq, n):
        kb.ts("dve", V(rstd), V(ssq), 1.0 / n, EPS, op0=ALU.mult, op1=ALU.add)
        kb.act(V(rstd), V(rstd), AF.Sqrt)
        kb.op("dve", lambda h: h.reciprocal(out=rstd.t[:], in_=rstd.t[:]), reads=[rstd], writes=[rstd])

    def rms_mod(self, kb, xt, G, SH, t1, out, ssq, rstd, junk, D):
        kb.act(V(junk), V(xt), AF.Square, accum=V(ssq))
        self.rstd_of(kb, rstd, ssq, D)
        kb.stt("dve", V(t1), V(xt), V(rstd), V(G), ALU.mult, ALU.mult)
        kb.tt("pool", V(out), V(t1), V(SH), ALU.add)

    def phase_ada(self):
        kb, I, S = self.kb, self.I, self.S
        D, KD = self.D, self.KD
        N6 = 6 * D
        NB = N6 // 512
        cv = kb.sb("cv", [128, KD, 2], F32)
        sv = kb.sb("sv", [128, KD, 2], BF16)
        kb.ld("sp", V(cv), V(I["cvec"]))
        kb.act(V(sv), V(cv), AF.Silu)
        wts = [kb.sb("wa%d" % i, [128, KD, 512], BF16) for i in range(2)]
        pss = [kb.ps("pa%d" % i, [128, 512]) for i in range(2)]
        bada = kb.sb("bada", [2, N6], F32)
        mrow = kb.sb("mrow", [2, N6], F32)
        for i in range(2):
            kb.ld("sp", V(bada), (I["b_ada"], I["b_ada"].t[i]))
            wsrc = I["w_ada"].t[i].rearrange("(k p) n -> p k n", p=128)
            for nb in range(NB):
                wt = wts[nb % 2]
                ps = pss[nb % 2]
                kb.ld("pool", V(wt), (I["w_ada"], wsrc[:, :, nb * 512:(nb + 1) * 512]))
                for k in range(KD):
                    kb.mm((ps, ps.t[0:2, :]), (sv, sv.t[:, k, :]), (wt, wt.t[:, k, :]), start=(k == 0), stop=(k == KD - 1))
                kb.tt("dve", (mrow, mrow.t[:, nb * 512:(nb + 1) * 512]), (ps, ps.t[0:2, :]),
                      (bada, bada.t[:, nb * 512:(nb + 1) * 512]), ALU.add)
            kb.ld("sp", V(S["MOD%d" % i]), V(mrow))

    def mod_vecs(self, kb, layer, cond, which, gain_row, tag):
        D = self.D
        M = self.S["MOD%d" % layer]
        base = 3 * which * D
        G = kb.sb("G" + tag, [128, D], F32)
        SH = kb.sb("SH" + tag, [128, D], F32)
        GT = kb.sb("GT" + tag, [128, D], F32)
        gn = kb.sb("gn" + tag, [128, D], F32)
        self.load_bcast(kb, V(SH), M.t[cond, base:base + D], M)
        self.load_bcast(kb, V(G), M.t[cond, base + D:base + 2 * D], M)
        self.load_bcast(kb, V(GT), M.t[cond, base + 2 * D:base + 3 * D], M)
        self.load_bcast(kb, V(gn), self.I["gains"].t[gain_row, :], self.I["gains"])
        kb.stt("dve", V(G), V(G), 1.0, V(gn), ALU.add, ALU.mult)
        return G, SH, GT, gn

    def phase_pool(self):
        kb, I, S = self.kb, self.I, self.S
        c = self.cfg
        D, KD, NT, NCT = self.D, self.KD, self.NT, self.NCT
        PG = D // 4
        PGC = PG // 128
        WINS = c["WINS"]
        NBLK = self.Bgrid.shape[1]
        NCLS = self.RCgrid.shape[1]
        bg = kb.sb("bg", [128, NBLK, 128], BF16)
        kb.ld("pool", V(bg), V(I["bgrid"]))
        rc = kb.sb("rc", [128, NCLS, 128], F32)
        kb.ld("sp", V(rc), V(I["rcgrid"]))
        bs = kb.sb("bs", [128, 4, NCT, NCT, 128], BF16)
        kb.ld("pool", V(bs), V(I["bseq"]))
        rcs = kb.sb("rcs", [128, 4, NCT, 128], F32)
        kb.ld("sp", V(rcs), V(I["rcseq"]))
        wp = kb.sb("wp", [128, 4, PGC, PG], BF16)
        kb.ld("pool", V(wp), (I["pool_w"], I["pool_w"].t.rearrange("j (c p) n -> p j c n", p=128)))
        ssq = kb.sb("ssq", [128, 1], F32)
        rstd = kb.sb("rstd", [128, 1], F32)
        junk = kb.sb("junk", [128, D], BF16)
        t1 = kb.sb("t1", [128, D], F32)
        xts = [kb.sb("xt%d" % i, [128, D], F32) for i in range(2)]
        xrs = [kb.sb("xr%d" % i, [128, D], F32) for i in range(2)]
        dT = kb.sb("dT", [128, KD, 128], BF16)
        yts = [kb.sb("yt%d" % i, [128, D], F32) for i in range(2)]
        pds = [kb.ps("pd%d" % i, [128, 512]) for i in range(2)]
        pys = [kb.ps("py%d" % i, [128, 512]) for i in range(2)]
        RING = 10
        hbig = kb.sb("hring", [128, RING, D], BF16)
        hring = [hbig.sub((slice(None), r, slice(None)), "h%d" % r) for r in range(RING)]

        for (cond, src, dst, ntile) in ((0, I["x"], S["X1"], NT), (1, I["ctx"], S["C1"], NCT)):
            tag = "m0%d" % cond
            with ExitStack() as sctx:
                old = kb.ctx
                kb.ctx = sctx
                G, SH, GT, gn = self.mod_vecs(kb, 0, cond, 0, 0, tag)
                self.load_bcast(kb, V(gn), I["gains"].t[5, :], I["gains"])
                kb.tt("dve", V(GT), V(GT), V(gn), ALU.mult)
                if cond == 0:
                    lead = max(max(o) for o in self.offs)
                else:
                    lead = NCT - 1
                npd = 0

                def make_h(i):
                    xt = xts[i % 2]
                    kb.ld("sp", V(xt), (src, src.t[i * 128:(i + 1) * 128, :]))
                    self.rms_mod(kb, xt, G, SH, t1, hring[i % RING], ssq, rstd, junk, D)

                for i in range(min(lead, ntile)):
                    make_h(i)
                for i in range(ntile):
                    if i + lead < ntile:
                        make_h(i + lead)
                    xr = xrs[i % 2]
                    kb.ld("sp", V(xr), (src, src.t[i * 128:(i + 1) * 128, :]))
                    yt = yts[i % 2]
                    for j in range(4):
                        if cond == 0:
                            nb = []
                            for d in self.offs[j]:
                                s = i + d
                                if s < 0 or s >= ntile:
                                    continue
                                if d == 0:
                                    blk = self.cls_of[(j, i)][0]
                                else:
                                    blk = self.bidx[(j, d)]
                                nb.append((s, (bg, bg.t[:, blk, :])))
                            rcap = (rc, rc.t[:, self.cls_of[(j, i)][1], :])
                        else:
                            nb = [(s, (bs, bs.t[:, j, s, i, :])) for s in range(ntile)]
                            rcap = (rcs, rcs.t[:, j, i, :])
                        pd = pds[npd % 2]
                        npd += 1
                        for cc in range(PGC):
                            ch = j * PG + cc * 128
                            for n, (s, bap) in enumerate(nb):
                                hb = hring[s % RING]
                                kb.mm((pd, pd.t[:, cc * 128:(cc + 1) * 128]), (hb, hb.t[:, ch:ch + 128]), bap,
                                      start=(n == 0), stop=(n == len(nb) - 1))
                        for cc in range(PGC):
                            kb.tt("dve", (dT, dT.t[:, j * PGC + cc, :]), (pd, pd.t[:, cc * 128:(cc + 1) * 128]), rcap, ALU.mult)
                        py = pys[j % 2]
                        for cc in range(PGC):
                            kb.mm((py, py.t[:, 0:PG]), (dT, dT.t[:, j * PGC + cc, :]), (wp, wp.t[:, j, cc, :]),
                                  start=(cc == 0), stop=(cc == PGC - 1))
                        kb.tt("dve", (yt, yt.t[:, j * PG:(j + 1) * PG]), (py, py.t[:, 0:PG]), (GT, GT.t[:, j * PG:(j + 1) * PG]), ALU.mult)
                    kb.tt("pool", V(yt), V(yt), V(xr), ALU.add)
                    kb.ld("sp", (dst, dst.t[i * 128:(i + 1) * 128, :]), V(yt), fill=True)
                kb.barrier()
                kb.ctx = old

    def phase_moe(self, layer):
        kb, I, S = self.kb, self.I, self.S
        c = self.cfg
        D, KD, NT, NCT, NE, NR = self.D, self.KD, self.NT, self.NCT, self.NE, self.NR
        NG, EG, DE, CAP = c["NG"], c["EG"], c["DE"], c["CAP"]
        DEC = DE // 128
        last = layer == 1
        if layer == 0:
            tiles = [(0, S["X1"], S["X2"], i) for i in range(NT)] + [(1, S["C1"], S["C2"], i) for i in range(NCT)]
        else:
            tiles = [(0, S["X3"], S["OUT"], i) for i in range(NT)]
        NTM = len(tiles)
        NS = c["NS"]
        SP0 = NE * CAP
        NROWS = SP0 + NS * 128
        breg = kb.pool_reg(NROWS - 1)
        SL = kb.sb("SL", [128, NTM, 2], I32)
        SL2 = kb.sb("SL2", [128, NTM, 2], I32)
        WT = kb.sb("WT", [128, NTM, 2], F32)
        RK = kb.sb("RK", [128, NTM, 2], F32)
        MS = kb.sb("MS", [128, NTM, 2], F32)
        BASE = kb.sb("BASE", [128, NE], F32)
        NBLK = kb.sb("NBLK", [128, NE], F32)
        ecap = kb.sb("ecap", [128, NE], F32)
        kb.ld("sp", V(ecap), V(I["ecap"]))

        with ExitStack() as sctx:
            kb.ctx = sctx
            mods = {0: self.mod_vecs(kb, layer, 0, 1, 2 + layer, "f0")}
            if layer == 0:
                mods[1] = self.mod_vecs(kb, layer, 1, 1, 2 + layer, "f1")
            ident = kb.sb("identf", [128, 128], F32)
            kb.ld("sp", V(ident), V(I["ident"]))
            wr = kb.sb("wr", [128, KD, NR], F32)
            kb.ld("sp", V(wr), (I["wr"], I["wr"].t[layer].rearrange("(k p) n -> p k n", p=128)))
            br = kb.sb("br", [128, NR], F32)
            kb.ld("sp", V(br), (I["br"], I["br"].t[layer]))
            tri = kb.sb("tri", [128, 128], F32)
            kb.ld("sp", V(tri), V(I["tri"]))
            ones = kb.sb("ones", [128, 128], F32)
            kb.memset("dve", V(ones), 1.0)
            Aall = kb.sb("Aall", [128, NTM, NE], F32)
            ssq = kb.sb("ssq", [128, 1], F32)
            rstd = kb.sb("rstd", [128, 1], F32)
            junk = kb.sb("junk", [128, D], BF16)
            t1 = kb.sb("t1", [128, D], F32)
            xts = [kb.sb("xt%d" % i, [128, D], F32) for i in range(2)]
            ff = kb.sb("ff", [128, D], F32)
            fbs = [kb.sb("fb%d" % i, [128, D], BF16) for i in range(2)]
            fT = kb.sb("fT", [128, KD, 128], F32)
            ptr = [kb.ps("ptr%d" % i, [128, 512]) for i in range(2)]
            plg = kb.ps("plg", [128, 512])
            prk = kb.ps("prk", [128, 512])
            lg = kb.sb("lg", [128, NR], F32)
            sm = kb.sb("sm", [128, 64], F32)
            gmask = kb.sb("gmask", [128, NG], F32)
            gexp = kb.sb("gexp", [128, NG], F32)
            lsel = kb.sb("lsel", [128, EG], F32)
            l2 = kb.sb("l2", [128, EG], F32)
            mk = [kb.sb("mk%d" % i, [128, EG], F32) for i in range(2)]
            oh = [kb.sb("oh%d" % i, [128, NE], F32) for i in range(2)]
            rk = kb.sb("rk", [128, NE], F32)
            prod = kb.sb("prod", [128, NE], F32)
            slf = kb.sb("slf", [128, 2], F32)

            def col(i):
                return (sm, sm.t[:, i:i + 1])

            for ti, (cond, src, dst, i) in enumerate(tiles):
                G, SH, GT, gn = mods[cond]
                xt = xts[ti % 2]
                kb.ld("sp", V(xt), (src, src.t[i * 128:(i + 1) * 128, :]))
                self.rms_mod(kb, xt, G, SH, t1, ff, ssq, rstd, junk, D)
                fb = fbs[ti % 2]
                kb.cp("act", V(fb), V(ff))
                for k in range(KD):
                    p = ptr[(k // 4) % 2]
                    kb.mm((p, p.t[:, (k % 4) * 128:(k % 4 + 1) * 128]), (ff, ff.t[:, k * 128:(k + 1) * 128]), V(ident), tr=True)
                    if k % 4 == 3 or k == KD - 1:
                        k0 = (k // 4) * 4
                        n = k - k0 + 1
                        kb.cp("dve", (fT, fT.t[:, k0:k0 + n, :]), (p, p.t[:, 0:n * 128].rearrange("p (a b) -> p a b", b=128)))
                for k in range(KD):
                    kb.mm((plg, plg.t[:, 0:NR]), (fT, fT.t[:, k, :]), (wr, wr.t[:, k, :]), start=(k == 0), stop=(k == KD - 1))
                kb.tt("dve", V(lg), (plg, plg.t[:, 0:NR]), V(br), ALU.add)
                kb.red("dve", col(0), (lg, lg.t[:, 0:NG]), ALU.max)
                kb.ts("dve", V(gmask), (lg, lg.t[:, 0:NG]), col(0), None, op0=ALU.is_equal)
                kb.ts("dve", col(1), col(0), -1.0, None, op0=ALU.mult)
                kb.act(V(gexp), (lg, lg.t[:, 0:NG]), AF.Exp, bias=col(1), scale=1.0)
                kb.red("dve", col(2), V(gexp), ALU.add)
                kb.op("dve", lambda h: h.reciprocal(out=sm.t[:, 3:4], in_=sm.t[:, 2:3]), reads=[sm], writes=[sm])
                for g in range(NG):
                    le = (lg, lg.t[:, NG + g * EG:NG + (g + 1) * EG])
                    if g == 0:
                        kb.ts("dve", V(lsel), le, (gmask, gmask.t[:, 0:1]), None, op0=ALU.mult)
                    else:
                        kb.stt("dve", V(lsel), le, (gmask, gmask.t[:, g:g + 1]), V(lsel), ALU.mult, ALU.add)
                kb.red("dve", col(4), V(lsel), ALU.max)
                kb.ts("dve", V(mk[0]), V(lsel), col(4), None, op0=ALU.is_equal)
                kb.stt("dve", V(l2), V(mk[0]), -1e30, V(lsel), ALU.mult, ALU.add)
                kb.red("dve", col(5), V(l2), ALU.max)
                kb.ts("dve", V(mk[1]), V(l2), col(5), None, op0=ALU.is_equal)
                kb.tt("dve", col(6), col(5), col(4), ALU.subtract)
                kb.act(col(7), col(6), AF.Exp)
                kb.ts("dve", col(8), col(7), 1.0, None, op0=ALU.add)
                kb.op("dve", lambda h: h.reciprocal(out=sm.t[:, 9:10], in_=sm.t[:, 8:9]), reads=[sm], writes=[sm])
                kb.tt("dve", (WT, WT.t[:, ti, 0:1]), col(3), col(9), ALU.mult)
                kb.tt("dve", (WT, WT.t[:, ti, 1:2]), (WT, WT.t[:, ti, 0:1]), col(7), ALU.mult)
                for kk in range(2):
                    for g in range(NG):
                        kb.ts("dve", (oh[kk], oh[kk].t[:, g * EG:(g + 1) * EG]), V(mk[kk]), (gmask, gmask.t[:, g:g + 1]), None, op0=ALU.mult)
                kb.tt("dve", (Aall, Aall.t[:, ti, :]), V(oh[0]), V(oh[1]), ALU.add)
                for tj in range(ti):
                    kb.mm((prk, prk.t[:, 0:NE]), V(ones), (Aall, Aall.t[:, tj, :]), start=(tj == 0), stop=False)
                kb.mm((prk, prk.t[:, 0:NE]), V(tri), (Aall, Aall.t[:, ti, :]), start=(ti == 0), stop=True)
                kb.cp("dve", V(rk), (prk, prk.t[:, 0:NE]))
                for kk in range(2):
                    kb.tt("dve", V(prod), V(oh[kk]), V(rk), ALU.mult)
                    kb.red("dve", col(10 + kk), V(prod), ALU.add)
                    kb.tt("dve", V(prod), V(oh[kk]), V(ecap), ALU.mult)
                    kb.red("dve", col(12 + kk), V(prod), ALU.add)
                    kb.ts("dve", col(14 + kk), col(10 + kk), float(CAP), 1e9, op0=ALU.is_ge, op1=ALU.mult)
                    kb.cp("dve", (RK, RK.t[:, ti, kk:kk + 1]), col(10 + kk))
                    kb.tt("dve", (MS, MS.t[:, ti, kk:kk + 1]), col(12 + kk), col(10 + kk), ALU.add)
                    kb.tt("dve", (slf, slf.t[:, kk:kk + 1]), (MS, MS.t[:, ti, kk:kk + 1]), col(14 + kk), ALU.add)
                kb.ts("dve", V(slf), V(slf), 2.0e9, None, op0=ALU.min)
                kb.cp("dve", (SL, SL.t[:, ti, :]), V(slf))
                kb.ld("sp", (S["FB"], S["FB"].t[ti * 128:(ti + 1) * 128, :]), V(fb), fill=True)
                for kk in range(2):
                    kb.dma("pool", lambda h, kk=kk, fb=fb, ti=ti: h.indirect_dma_start(
                        out=S["XS"].t[:, :], out_offset=bass.IndirectOffsetOnAxis(ap=SL.t[:, ti, kk:kk + 1], axis=0),
                        in_=fb.t[:, :], in_offset=None, bounds_check=breg, oob_is_err=False),
                        reads=[fb, SL], writes=[S["XS"]], fill=True)
            for tj in range(NTM):
                kb.mm((prk, prk.t[:, 0:NE]), V(ones), (Aall, Aall.t[:, tj, :]), start=(tj == 0), stop=(tj == NTM - 1))
            kb.ts("dve", V(rk), (prk, prk.t[:, 0:NE]), -float(CAP), 0.0, op0=ALU.add, op1=ALU.max)
            kb.ts("dve", V(NBLK), V(rk), 0.0, None, op0=ALU.is_gt)
            for j in range(1, NS):
                kb.stt("dve", V(NBLK), V(rk), float(128 * j), V(NBLK), ALU.is_gt, ALU.add)
            kb.op("dve", lambda h: h.tensor_tensor_scan(out=BASE.t[:], data0=ones.t[:, 0:NE], data1=NBLK.t[:], initial=0.0,
                                                          op0=ALU.mult, op1=ALU.add), reads=[ones, NBLK], writes=[BASE])
            kb.tt("dve", V(BASE), V(BASE), V(NBLK), ALU.subtract)
            for ti in range(NTM):
                fb = fbs[ti % 2]
                kb.ld("sp", V(fb), (S["FB"], S["FB"].t[ti * 128:(ti + 1) * 128, :]))
                for kk in range(2):
                    rkc = (RK, RK.t[:, ti, kk:kk + 1])
                    msc = (MS, MS.t[:, ti, kk:kk + 1])
                    kb.tt("dve", col(19), msc, rkc, ALU.subtract)
                    kb.ts("dve", V(oh[0]), V(ecap), col(19), None, op0=ALU.is_equal)
                    kb.tt("dve", V(prod), V(oh[0]), V(BASE), ALU.mult)
                    kb.red("dve", col(20), V(prod), ALU.add)
                    kb.ts("dve", col(21), rkc, float(CAP), None, op0=ALU.is_ge)
                    kb.stt("dve", col(22), col(20), 128.0, rkc, ALU.mult, ALU.add)
                    kb.ts("dve", col(22), col(22), float(SP0 - CAP), None, op0=ALU.add)
                    kb.ts("dve", col(23), col(22), float(NROWS), None, op0=ALU.is_lt)
                    kb.tt("dve", col(23), col(23), col(21), ALU.mult)
                    kb.ts("dve", col(24), col(23), -1e9, 1e9, op0=ALU.mult, op1=ALU.add)
                    kb.tt("dve", col(25), col(22), col(23), ALU.mult)
                    kb.tt("dve", (slf, slf.t[:, kk:kk + 1]), col(25), col(24), ALU.add)
                    kb.ts("dve", col(26), col(21), -1.0, 1.0, op0=ALU.mult, op1=ALU.add)
                    kb.tt("dve", col(27), msc, col(26), ALU.mult)
                    kb.tt("dve", col(27), col(27), col(25), ALU.add)
                    kb.tt("dve", col(28), col(21), col(23), ALU.subtract)
                    kb.stt("dve", (sm, sm.t[:, 30 + kk:31 + kk]), col(28), 1e9, col(27), ALU.mult, ALU.add)
                kb.cp("dve", (SL2, SL2.t[:, ti, :]), V(slf))
                kb.cp("dve", (SL, SL.t[:, ti, :]), (sm, sm.t[:, 30:32]))
                for kk in range(2):
                    kb.dma("pool", lambda h, kk=kk, fb=fb, ti=ti: h.indirect_dma_start(
                        out=S["XS"].t[:, :], out_offset=bass.IndirectOffsetOnAxis(ap=SL2.t[:, ti, kk:kk + 1], axis=0),
                        in_=fb.t[:, :], in_offset=None, bounds_check=breg, oob_is_err=False),
                        reads=[fb, SL2], writes=[S["XS"]], fill=True)
            kb.barrier()
            kb.ctx = old = None

        with ExitStack() as sctx:
            kb.ctx = sctx
            NBK = CAP // 128
            NHB = (CAP + 511) // 512
            identb = kb.sb("identb", [128, 128], BF16)
            kb.ld("pool", V(identb), V(I["ident"]))
            xsT = [kb.sb("xsT%d" % i, [128, KD, CAP], BF16) for i in range(2)]
            HT = [kb.sb("HT%d" % i, [128, DEC, CAP], BF16) for i in range(2)]
            xrows = [kb.sb("xrow%d" % i, [128, D], BF16) for i in range(2)]
            wgs = [kb.sb("wg%d" % i, [128, KD, 2, 128], BF16) for i in range(3)]
            wds = [kb.sb("wd%d" % i, [128, DEC, 512], BF16) for i in range(3)]
            sas = [kb.sb("sa%d" % i, [128, 512], F32) for i in range(2)]
            yos = [kb.sb("yo%d" % i, [128, 512], F32) for i in range(3)]
            ptb = [kb.ps("ptb%d" % i, [128, 512], BF16) for i in range(2)]
            pab = [kb.ps("pab%d" % i, [128, 512]) for i in range(4)]
            pyy = [kb.ps("pyy%d" % i, [128, 512]) for i in range(2)]
            nwg = nwd = nsa = nyo = npy = npab = nxr = 0
            for e in range(NE):
                xT = xsT[e % 2]
                hT = HT[e % 2]
                for bk in range(NBK):
                    xr = xrows[nxr % 2]
                    nxr += 1
                    r0 = e * CAP + bk * 128
                    kb.ld("sp", V(xr), (S["XS"], S["XS"].t[r0:r0 + 128, :]))
                    for k in range(KD):
                        p = ptb[(k // 4) % 2]
                        kb.mm((p, p.t[:, (k % 4) * 128:(k % 4 + 1) * 128]), (xr, xr.t[:, k * 128:(k + 1) * 128]), V(identb), tr=True)
                        if k % 4 == 3 or k == KD - 1:
                            k0 = (k // 4) * 4
                            n = k - k0 + 1
                            kb.cp("act" if (k // 4) % 2 else "dve", (xT, xT.t[:, k0:k0 + n, bk * 128:(bk + 1) * 128]),
                                  (p, p.t[:, 0:n * 128].rearrange("p (a b) -> p a b", b=128)))
                wsrc = I["w_gu"].t[layer, e].rearrange("(k p) n -> p k n", p=128)
                for j in range(DEC):
                    wg = wgs[nwg % 3]
                    nwg += 1
                    kb.ld("pool", (wg, wg.t[:, :, 0, :]), (I["w_gu"], wsrc[:, :, j * 128:(j + 1) * 128]))
                    kb.ld("pool", (wg, wg.t[:, :, 1, :]), (I["w_gu"], wsrc[:, :, DE + j * 128:DE + (j + 1) * 128]), fill=True)
                    for hb in range(NHB):
                        c0 = hb * 512
                        n = min(512, CAP - c0)
                        pa = pab[npab % 4]
                        pb = pab[(npab + 1) % 4]
                        npab += 2
                        for k in range(KD):
                            kb.mm((pa, pa.t[:, 0:n]), (wg, wg.t[:, k, 0, :]), (xT, xT.t[:, k, c0:c0 + n]), start=(k == 0), stop=(k == KD - 1))
                        for k in range(KD):
                            kb.mm((pb, pb.t[:, 0:n]), (wg, wg.t[:, k, 1, :]), (xT, xT.t[:, k, c0:c0 + n]), start=(k == 0), stop=(k == KD - 1))
                        sa = sas[nsa % 2]
                        nsa += 1
                        kb.act((sa, sa.t[:, 0:n]), (pa, pa.t[:, 0:n]), AF.Silu)
                        kb.tt("dve", (hT, hT.t[:, j, c0:c0 + n]), (sa, sa.t[:, 0:n]), (pb, pb.t[:, 0:n]), ALU.mult)
                dsrc = I["w_dn"].t[layer, e].rearrange("(k p) n -> p k n", p=128)
                for nb in range(D // 512):
                    wd = wds[nwd % 3]
                    nwd += 1
                    kb.ld("pool", V(wd), (I["w_dn"], dsrc[:, :, nb * 512:(nb + 1) * 512]))
                    for bk in range(NBK):
                        py = pyy[npy % 2]
                        npy += 1
                        for j in range(DEC):
                            kb.mm(V(py), (hT, hT.t[:, j, bk * 128:(bk + 1) * 128]), (wd, wd.t[:, j, :]), start=(j == 0), stop=(j == DEC - 1))
                        yo = yos[nyo % 3]
                        nyo += 1
                        kb.cp("act" if nyo % 2 else "dve", V(yo), V(py))
                        r0 = e * CAP + bk * 128
                        kb.ld("sp", (S["YS"], S["YS"].t[r0:r0 + 128, nb * 512:(nb + 1) * 512]), V(yo), fill=True)
            WROW = max(2 * DE, D)
            wring = [kb.sb("wring%d" % i, [128, WROW], BF16) for i in range(3)]
            xTs = kb.sb("xTs", [128, KD, 128], BF16)
            Hs = kb.sb("Hs", [128, DE], BF16)
            sas_ = kb.sb("sas", [128, DE], F32)
            HTs = kb.sb("HTs", [128, DEC, 128], BF16)
            yfull = kb.sb("yfull", [128, D], F32)
            pcol = kb.sb("pcol", [128, 1], F32)
            kb.ld("sp", V(pcol), V(I["pcol"]))
            eio = kb.sb("eio", [128, NE], F32)
            kb.ts("dve", V(eio), V(ecap), 1.0 / CAP, None, op0=ALU.mult)
            t_a = kb.sb("t_a", [128, NE], F32)
            t_b = kb.sb("t_b", [128, NE], F32)
            scol = kb.sb("scol", [128, 4], F32)
            idxg = kb.sb("idxg", [128, NS, 2], I32)
            w_gu2d = I["w_gu"].t.rearrange("l e d n -> (l e d) n")
            w_dn2d = I["w_dn"].t.rearrange("l e d n -> (l e d) n")
            NGB = (2 * DE + 511) // 512
            NDB = D // 512
            nring = 0
            for b in range(NS):
                kb.ts("dve", V(t_a), V(BASE), float(b), None, op0=ALU.is_le)
                kb.tt("dve", V(t_b), V(BASE), V(NBLK), ALU.add)
                kb.ts("dve", V(t_b), V(t_b), float(b), None, op0=ALU.is_gt)
                kb.tt("dve", V(t_a), V(t_a), V(t_b), ALU.mult)
                kb.tt("dve", V(t_a), V(t_a), V(eio), ALU.mult)
                kb.red("dve", (scol, scol.t[:, 0:1]), V(t_a), ALU.add)
                kb.stt("dve", (scol, scol.t[:, 1:2]), (scol, scol.t[:, 0:1]), float(D), V(pcol), ALU.mult, ALU.add)
                kb.stt("dve", (scol, scol.t[:, 2:3]), (scol, scol.t[:, 0:1]), float(DE), V(pcol), ALU.mult, ALU.add)
                if layer > 0:
                    kb.ts("dve", (scol, scol.t[:, 1:2]), (scol, scol.t[:, 1:2]), float(layer * NE * D), None, op0=ALU.add)
                    kb.ts("dve", (scol, scol.t[:, 2:3]), (scol, scol.t[:, 2:3]), float(layer * NE * DE), None, op0=ALU.add)
                kb.cp("dve", (idxg, idxg.t[:, b, :]), (scol, scol.t[:, 1:3]))
                xr = xrows[nxr % 2]
                nxr += 1
                r0 = SP0 + b * 128
                kb.ld("sp", V(xr), (S["XS"], S["XS"].t[r0:r0 + 128, :]))
                for k in range(KD):
                    p = ptb[(k // 4) % 2]
                    kb.mm((p, p.t[:, (k % 4) * 128:(k % 4 + 1) * 128]), (xr, xr.t[:, k * 128:(k + 1) * 128]), V(identb), tr=True)
                    if k % 4 == 3 or k == KD - 1:
                        k0 = (k // 4) * 4
                        n = k - k0 + 1
                        kb.cp("act" if (k // 4) % 2 else "dve", (xTs, xTs.t[:, k0:k0 + n, :]),
                              (p, p.t[:, 0:n * 128].rearrange("p (a b) -> p a b", b=128)))
                for k in range(KD):
                    wk = wring[nring % 3]
                    nring += 1
                    kb.dma("pool", lambda h, wk=wk, b=b, k=k: h.indirect_dma_start(
                        out=wk.t[:, 0:2 * DE], out_offset=None, in_=w_gu2d,
                        in_offset=bass.IndirectOffsetOnAxis(ap=idxg.t[:, b, 0:1], axis=0), element_offset=k * 128 * 2 * DE),
                        reads=[I["w_gu"], idxg], writes=[wk])
                    for g4 in range(NGB):
                        n = min(512, 2 * DE - g4 * 512)
                        kb.mm((pab[g4], pab[g4].t[:, 0:n]), (xTs, xTs.t[:, k, :]), (wk, wk.t[:, g4 * 512:g4 * 512 + n]),
                              start=(k == 0), stop=(k == KD - 1))
                for c0 in range(0, DE, 512):
                    n = min(512, DE - c0)
                    ba, oa = c0 // 512, c0 % 512
                    bb_, ob = (DE + c0) // 512, (DE + c0) % 512
                    kb.act((sas_, sas_.t[:, c0:c0 + n]), (pab[ba], pab[ba].t[:, oa:oa + n]), AF.Silu)
                    kb.tt("dve", (Hs, Hs.t[:, c0:c0 + n]), (sas_, sas_.t[:, c0:c0 + n]), (pab[bb_], pab[bb_].t[:, ob:ob + n]), ALU.mult)
                for j in range(DEC):
                    p = ptb[(j // 4) % 2]
                    kb.mm((p, p.t[:, (j % 4) * 128:(j % 4 + 1) * 128]), (Hs, Hs.t[:, j * 128:(j + 1) * 128]), V(identb), tr=True)
                    if j % 4 == 3 or j == DEC - 1:
                        j0 = (j // 4) * 4
                        n = j - j0 + 1
                        kb.cp("act" if (j // 4) % 2 else "dve", (HTs, HTs.t[:, j0:j0 + n, :]),
                              (p, p.t[:, 0:n * 128].rearrange("p (a b) -> p a b", b=128)))
                for j in range(DEC):
                    wk = wring[nring % 3]
                    nring += 1
                    kb.dma("pool", lambda h, wk=wk, b=b, j=j: h.indirect_dma_start(
                        out=wk.t[:, 0:D], out_offset=None, in_=w_dn2d,
                        in_offset=bass.IndirectOffsetOnAxis(ap=idxg.t[:, b, 1:2], axis=0), element_offset=j * 128 * D),
                        reads=[I["w_dn"], idxg], writes=[wk])
                    for nb in range(NDB):
                        kb.mm(V(pab[nb]), (HTs, HTs.t[:, j, :]), (wk, wk.t[:, nb * 512:(nb + 1) * 512]), start=(j == 0), stop=(j == DEC - 1))
                for nb in range(NDB):
                    kb.cp("act" if nb % 2 else "dve", (yfull, yfull.t[:, nb * 512:(nb + 1) * 512]), V(pab[nb]))
                kb.ld("sp", (S["YS"], S["YS"].t[r0:r0 + 128, :]), V(yfull), fill=True)
            kb.barrier()
            kb.ctx = None

        with ExitStack() as sctx:
            kb.ctx = sctx
            gts = {0: kb.sb("GTc0", [128, D], F32)}
            M = S["MOD%d" % layer]
            self.load_bcast(kb, V(gts[0]), M.t[0, 5 * D:6 * D], M)
            if layer == 0:
                gts[1] = kb.sb("GTc1", [128, D], F32)
                self.load_bcast(kb, V(gts[1]), M.t[1, 5 * D:6 * D], M)
            if last:
                gfin = kb.sb("gfin", [128, D], F32)
                self.load_bcast(kb, V(gfin), I["gains"].t[4, :], I["gains"])
                ssq = kb.sb("ssq", [128, 1], F32)
                rstd = kb.sb("rstd", [128, 1], F32)
                junk = kb.sb("junk", [128, D], BF16)
            ygs = [[kb.sb("yg%d_%d" % (a, b), [128, D], F32) for b in range(2)] for a in range(2)]
            xts = [kb.sb("xt%d" % i, [128, D], F32) for i in range(2)]
            acc = [kb.sb("acc%d" % i, [128, D], F32) for i in range(2)]
            for ti, (cond, src, dst, i) in enumerate(tiles):
                xt = xts[ti % 2]
                kb.ld("sp", V(xt), (src, src.t[i * 128:(i + 1) * 128, :]))
                yg = ygs[ti % 2]
                for kk in range(2):
                    kb.memset("pool", V(yg[kk]), 0.0)
                    kb.dma("pool", lambda h, kk=kk, yg=yg, ti=ti: h.indirect_dma_start(
                        out=yg[kk].t[:, :], out_offset=None, in_=S["YS"].t[:, :],
                        in_offset=bass.IndirectOffsetOnAxis(ap=SL.t[:, ti, kk:kk + 1], axis=0),
                        bounds_check=breg, oob_is_err=False),
                        reads=[S["YS"], SL], writes=[yg[kk]])
                a = acc[ti % 2]
                kb.ts("dve", V(a), V(yg[0]), (WT, WT.t[:, ti, 0:1]), None, op0=ALU.mult)
                kb.stt("dve", V(a), V(yg[1]), (WT, WT.t[:, ti, 1:2]), V(a), ALU.mult, ALU.add)
                kb.tt("pool", V(a), V(a), V(gts[cond]), ALU.mult)
                kb.tt("dve", V(a), V(a), V(xt), ALU.add)
                if last:
                    kb.act(V(junk), V(a), AF.Square, accum=V(ssq))
                    self.rstd_of(kb, rstd, ssq, D)
                    kb.stt("dve", V(a), V(a), V(rstd), V(gfin), ALU.mult, ALU.mult)
                kb.ld("sp", (dst, dst.t[i * 128:(i + 1) * 128, :]), V(a), fill=True)
            kb.barrier()
            kb.ctx = None

    def phase_hgrn(self):
        kb, I, S = self.kb, self.I, self.S
        c = self.cfg
        D, KD, NT, NCT, L, LC = self.D, self.KD, self.NT, self.NCT, self.L, self.LC
        NH = c["NH"]
        SEG = self.SEG
        NSEG = L // SEG

        with ExitStack() as sctx:
            kb.ctx = sctx
            identb = kb.sb("identb", [128, 128], BF16)
            kb.ld("pool", V(identb), V(I["ident"]))
            ssq = kb.sb("ssq", [128, 1], F32)
            rstd = kb.sb("rstd", [128, 1], F32)
            junk = kb.sb("junk", [128, D], BF16)
            t1 = kb.sb("t1", [128, D], F32)
            xts = [kb.sb("xt%d" % i, [128, D], F32) for i in range(2)]
            hbs = [kb.sb("hb%d" % i, [128, D], BF16) for i in range(2)]
            hTg = [kb.sb("hTg%d" % i, [128, KD, 512], BF16) for i in range(2)]
            ptb = [kb.ps("ptb%d" % i, [128, 512], BF16) for i in range(2)]
            for (cond, src, dstT, ntile) in ((0, S["X2"], S["HXT"], NT), (1, S["C2"], S["HCT"], NCT)):
                with ExitStack() as s2:
                    kb.ctx = s2
                    G, SH, GT, gn = self.mod_vecs(kb, 1, cond, 0, 1, "h%d" % cond)
                    dview = dstT.t.rearrange("(k p) t -> p k t", p=128)
                    ng = 0
                    for i in range(ntile):
                        xt = xts[i % 2]
                        kb.ld("sp", V(xt), (src, src.t[i * 128:(i + 1) * 128, :]))
                        hb = hbs[i % 2]
                        self.rms_mod(kb, xt, G, SH, t1, hb, ssq, rstd, junk, D)
                        hg = hTg[ng % 2]
                        sub = i % 4
                        for k in range(KD):
                            p = ptb[(k // 4) % 2]
                            kb.mm((p, p.t[:, (k % 4) * 128:(k % 4 + 1) * 128]), (hb, hb.t[:, k * 128:(k + 1) * 128]), V(identb), tr=True)
                            if k % 4 == 3 or k == KD - 1:
                                k0 = (k // 4) * 4
                                n = k - k0 + 1
                                kb.cp("act" if (k // 4) % 2 else "dve", (hg, hg.t[:, k0:k0 + n, sub * 128:(sub + 1) * 128]),
                                      (p, p.t[:, 0:n * 128].rearrange("p (a b) -> p a b", b=128)))
                        if sub == 3 or i == ntile - 1:
                            t0 = (i // 4) * 512
                            n = (sub + 1) * 128
                            kb.ld("sp", (dstT, dview[:, :, t0:t0 + n]), (hg, hg.t[:, :, 0:n]), fill=True)
                            ng += 1
                    kb.barrier()
                    kb.ctx = sctx
            kb.barrier()
            kb.ctx = None

        with ExitStack() as sctx:
            kb.ctx = sctx
            NCS = SEG // 128
            identb = kb.sb("identb", [128, 128], BF16)
            kb.ld("pool", V(identb), V(I["ident"]))
            onesb = kb.sb("onesb", [128, 128], BF16)
            kb.memset("pool", V(onesb), 1.0)
            m128 = kb.sb("m128", [128, SEG], F32)
            kb.ld("sp", V(m128), V(I["mask128"]))
            m32 = kb.sb("m32", [128, SEG], F32)
            kb.ld("sp", V(m32), V(I["mask32"]))
            trim = kb.sb("trim", [128, 2, 128], F32)
            kb.ld("sp", V(trim), V(I["trimask"]))
            hn = kb.sb("hn", [128, 1], F32)
            kb.ld("sp", V(hn), V(I["hnorm"]))
            lbl = kb.sb("lbl", [128, 2, 2, NH], F32)
            kb.ld("sp", V(lbl), V(I["lbl"]))
            LB = kb.sb("LB", [128, 2, NH], F32)
            OML = kb.sb("OML", [128, 2, NH], F32)
            kb.tt("dve", V(LB), (lbl, lbl.t[:, 1]), (lbl, lbl.t[:, 0]), ALU.subtract)
            kb.act(V(LB), V(LB), AF.Sigmoid)
            kb.ts("dve", V(OML), V(LB), -1.0, 1.0, op0=ALU.mult, op1=ALU.add)
            NOML = kb.sb("NOML", [128, 2, NH], F32)
            kb.ts("dve", V(NOML), V(OML), -1.0, None, op0=ALU.mult)
            Sball = kb.sb("Sball", [128, SEG // 128, 128], BF16)
            wh = kb.sb("wh", [128, KD, 5, 128], BF16)
            HW = 256
            hts = [kb.sb("ht%d" % i, [128, KD, HW], BF16) for i in range(2)]
            hcT = kb.sb("hcT", [128, KD, LC], BF16)
            qT = kb.sb("qT", [128, L], BF16)
            vtok = kb.sb("vtok", [128, NT, 128], BF16)
            vctok = kb.sb("vctok", [128, NCT, 128], BF16)
            OT = kb.sb("OT", [128, L], F32)
            zTs = [kb.sb("zT%d" % i, [128, SEG], F32) for i in range(2)]
            lf = kb.sb("lf", [128, SEG], F32)
            kf = kb.sb("kf", [128, SEG], F32)
            bb = kb.sb("bb", [128, SEG], F32)
            bl = kb.sb("bl", [128, SEG], F32)
            cEb = kb.sb("cEb", [128, SEG], F32)
            clb = kb.sb("clb", [128, SEG], F32)
            rr = kb.sb("rr", [128, SEG], F32)
            e1 = kb.sb("e1", [128, SEG], F32)
            qe = kb.sb("qe", [128, SEG], BF16)
            qE = kb.sb("qE", [128, SEG], BF16)
            ke = [[kb.sb("ke%d_%d" % (d, i), [128, SEG], BF16) for i in range(4)] for d in range(2)]
            for d in range(2):
                for i in range(4):
                    kb.memset("pool", V(ke[d][i]), 0.0)
            kdT = kb.sb("kdT", [128, SEG], BF16)
            kdT_f = kb.sb("kdTf", [128, SEG], F32)
            kdtok = kb.sb("kdtok", [128, NCS, 128], BF16)
            At = kb.sb("At", [128, NCS, 128], BF16)
            sgTs = [kb.sb("sgT%d" % i, [128, SEG], BF16) for i in range(2)]
            rr2 = kb.sb("rr2", [128, SEG], F32)
            eb = kb.sb("eb", [128, NCS], F32)
            Sst = [kb.sb("S%d" % d, [128, 128], F32) for d in range(2)]
            Sbf = [kb.sb("Sb%d" % d, [128, 128], BF16) for d in range(2)]
            osum = kb.sb("osum", [128, 512], F32)
            osq = kb.sb("osq", [128, 512], BF16)
            rs = kb.sb("rs", [128, 512], F32)
            yo = [kb.sb("yo%d" % i, [128, 512], BF16) for i in range(2)]
            pz = [kb.ps("pz%d" % i, [128, 512]) for i in range(2)]
            pv = kb.ps("pv", [128, 512])
            pA = kb.ps("pA", [128, 512])
            ptk = kb.ps("ptk", [128, 512], BF16)
            po = kb.ps("po", [128, 512])
            pS = kb.ps("pS", [128, 512])
            pn = kb.ps("pn", [128, 512])
            cnt = {"pz": 0, "yo": 0, "ht": 0, "z": 0}
            hxv = S["HXT"].t.rearrange("(k p) t -> p k t", p=128)
            hcv = S["HCT"].t.rearrange("(k p) t -> p k t", p=128)
            ytv = S["YT"].t.rearrange("(h p) t -> p h t", p=128)

            def v3(b, n, inner=128):
                return b.t[:, 0:n].rearrange("p (c s) -> p c s", s=inner)

            def proj_fm(hT, n, seg, evac):
                p = pz[cnt["pz"] % 2]
                cnt["pz"] += 1
                for k in range(KD):
                    kb.mm((p, p.t[:, 0:n]), (wh, wh.t[:, k, seg, :]), (hT, hT.t[:, k, 0:n]), start=(k == 0), stop=(k == KD - 1))
                evac((p, p.t[:, 0:n]))

            def proj_v(hT, n, dst, c0):
                nsub = n // 128
                for j in range(nsub):
                    for k in range(KD):
                        kb.mm((pv, pv.t[:, j * 128:(j + 1) * 128]), (hT, hT.t[:, k, j * 128:(j + 1) * 128]), (wh, wh.t[:, k, 4, :]),
                              start=(k == 0), stop=(k == KD - 1))
                kb.cp("act", (dst, dst.t[:, c0:c0 + nsub, :]), (pv, pv.t[:, 0:n].rearrange("p (a b) -> p a b", b=128)))

            def gla_seg(h, d, nch, zsrc, qsrc, vt, vc0, need_o, t0, sgT=None):
                n = nch * 128
                S_ = Sst[d]
                lbv = (LB, LB.t[:, d, h:h + 1])
                omv = (OML, OML.t[:, d, h:h + 1])
                nomv = (NOML, NOML.t[:, d, h:h + 1])
                kb.act((e1, e1.t[:, 0:n]), (zsrc, zsrc.t[:, 0:n]), AF.Sigmoid)
                kb.act((lf, lf.t[:, 0:n]), (e1, e1.t[:, 0:n]), AF.Ln, bias=lbv, scale=omv)
                kb.ts("pool", (kf, kf.t[:, 0:n]), (e1, e1.t[:, 0:n]), nomv, omv, op0=ALU.mult, op1=ALU.add)
                kb.op("dve", lambda hh: hh.tensor_tensor_scan(out=bb.t[:, 0:n], data0=m128.t[:, 0:n], data1=lf.t[:, 0:n],
                                                               initial=0.0, op0=ALU.mult, op1=ALU.add),
                      reads=[m128, lf], writes=[bb])
                if need_o:
                    kb.op("dve", lambda hh: hh.tensor_tensor_scan(out=bl.t[:, 0:n], data0=m32.t[:, 0:n], data1=lf.t[:, 0:n],
                                                                   initial=0.0, op0=ALU.mult, op1=ALU.add),
                          reads=[m32, lf], writes=[bl])
                if d == 0:
                    cE, cl = bb, bl
                    last = 127
                else:
                    cE, cl = cEb, clb
                    last = 0
                    kb.tt("dve", (rr, rr.t[:, 0:n]), (lf, lf.t[:, 0:n]), (bb, bb.t[:, 0:n]), ALU.subtract)
                    kb.tt("dve", (cEb, v3(cEb, n)), (rr, v3(rr, n)), (bb, v3(bb, n)[:, :, 127:128].to_broadcast([128, nch, 128])), ALU.add)
                    if need_o:
                        kb.tt("dve", (rr, rr.t[:, 0:n]), (lf, lf.t[:, 0:n]), (bl, bl.t[:, 0:n]), ALU.subtract)
                        kb.tt("dve", (clb, v3(clb, n, 32)), (rr, v3(rr, n, 32)),
                              (bl, v3(bl, n, 32)[:, :, 31:32].to_broadcast([128, nch * 4, 32])), ALU.add)
                cE3 = v3(cE, n)
                if need_o:
                    kb.act((e1, e1.t[:, 0:n]), (cl, cl.t[:, 0:n]), AF.Exp)
                    kb.tt("pool", (qe, qe.t[:, 0:n]), (e1, e1.t[:, 0:n]), qsrc, ALU.mult)
                    kb.act((kdT_f, kdT_f.t[:, 0:n]), (cE, cE.t[:, 0:n]), AF.Exp)
                    kb.tt("pool", (qE, qE.t[:, 0:n]), (kdT_f, kdT_f.t[:, 0:n]), qsrc, ALU.mult)
                    for i4 in range(4):
                        if d == 0:
                            r0, r1 = 0, 32 * (i4 + 1)
                            ref = None if i4 == 0 else cE3[:, :, 32 * i4 - 1:32 * i4]
                        else:
                            r0, r1 = 32 * i4, 128
                            ref = None if i4 == 3 else cE3[:, :, 32 * (i4 + 1):32 * (i4 + 1) + 1]
                        nr = r1 - r0
                        rb = rr if i4 % 2 == 0 else rr2
                        rv = v3(rb, n)[:, :, r0:r1]
                        if ref is None:
                            kb.ts("dve", (rb, rv), (cE, cE3[:, :, r0:r1]), -1.0, None, op0=ALU.mult)
                        else:
                            kb.stt("dve", (rb, rv), (cE, cE3[:, :, r0:r1]), -1.0, (cE, ref.to_broadcast([128, nch, nr])), ALU.mult, ALU.add)
                        kb.act((rb, rv), (rb, rv), AF.Exp)
                        kb.tt("pool", (ke[d][i4], v3(ke[d][i4], n)[:, :, r0:r1]), (rb, rv), (kf, v3(kf, n)[:, :, r0:r1]), ALU.mult)
                    for c0 in range(0, nch, 4):
                        nc4 = min(4, nch - c0)
                        for cc in range(nc4):
                            ch = c0 + cc
                            for i4 in range(4):
                                kb.mm((pA, pA.t[:, cc * 128 + 32 * i4:cc * 128 + 32 * i4 + 32]),
                                      (ke[d][i4], ke[d][i4].t[:, ch * 128:(ch + 1) * 128]),
                                      (qe, qe.t[:, ch * 128 + 32 * i4:ch * 128 + 32 * i4 + 32]))
                        kb.tt("dve", (At, At.t[:, c0:c0 + nc4, :]), (pA, pA.t[:, 0:nc4 * 128].rearrange("p (a b) -> p a b", b=128)),
                              (trim, trim.t[:, d:d + 1, :].to_broadcast([128, nc4, 128])), ALU.mult)
                kb.stt("dve", (rr, v3(rr, n)), (cE, cE3), -1.0, (cE, cE3[:, :, last:last + 1].to_broadcast([128, nch, 128])), ALU.mult, ALU.add)
                kb.act((rr, rr.t[:, 0:n]), (rr, rr.t[:, 0:n]), AF.Exp)
                kb.tt("pool", (kdT, kdT.t[:, 0:n]), (rr, rr.t[:, 0:n]), (kf, kf.t[:, 0:n]), ALU.mult)
                kb.act((eb, eb.t[:, 0:nch]), (cE, cE3[:, :, last]), AF.Exp)
                for c0 in range(0, nch, 4):
                    nc4 = min(4, nch - c0)
                    for cc in range(nc4):
                        ch = c0 + cc
                        kb.mm((ptk, ptk.t[:, cc * 128:(cc + 1) * 128]), (kdT, kdT.t[:, ch * 128:(ch + 1) * 128]), V(identb), tr=True)
                    kb.cp("act", (kdtok, kdtok.t[:, c0:c0 + nc4, :]), (ptk, ptk.t[:, 0:nc4 * 128].rearrange("p (a b) -> p a b", b=128)))
                order = list(range(nch)) if d == 0 else list(range(nch - 1, -1, -1))
                for g0 in range(0, nch, 4):
                    grp = order[g0:g0 + 4]
                    for gi, ch in enumerate(grp):
                        kb.mm((pS, pS.t[:, gi * 128:(gi + 1) * 128]), (kdtok, kdtok.t[:, ch, :]), (vt, vt.t[:, vc0 + ch, :]))
                    for gi, ch in enumerate(grp):
                        if need_o:
                            kb.cp("dve", (Sball, Sball.t[:, ch, :]), V(S_))
                        kb.stt("dve", V(S_), V(S_), (eb, eb.t[:, ch:ch + 1]), (pS, pS.t[:, gi * 128:(gi + 1) * 128]), ALU.mult, ALU.add)
                if need_o:
                    for g0 in range(0, nch, 4):
                        grp = order[g0:g0 + 4]
                        for ch in grp:
                            slot = ch % 4
                            pov = (po, po.t[:, slot * 128:(slot + 1) * 128])
                            kb.mm(pov, (vt, vt.t[:, vc0 + ch, :]), (At, At.t[:, ch, :]), start=True, stop=False)
                            kb.mm(pov, (Sball, Sball.t[:, ch, :]), (qE, qE.t[:, ch * 128:(ch + 1) * 128]), start=False, stop=True)
                        c_lo = min(grp)
                        ng_ = len(grp) * 128
                        tgg = t0 + c_lo * 128
                        if d == 0:
                            kb.cp("act", (OT, OT.t[:, tgg:tgg + ng_]), (po, po.t[:, 0:ng_]))
                        else:
                            kb.tt("dve", (osum, osum.t[:, 0:ng_]), (po, po.t[:, 0:ng_]), (OT, OT.t[:, tgg:tgg + ng_]), ALU.add)
                            kb.act((osq, osq.t[:, 0:ng_]), (osum, osum.t[:, 0:ng_]), AF.Square)
                            kb.mm((pn, pn.t[:, 0:ng_]), V(onesb), (osq, osq.t[:, 0:ng_]))
                            kb.ts("dve", (rs, rs.t[:, 0:ng_]), (pn, pn.t[:, 0:ng_]), 1.0 / 128, EPS, op0=ALU.mult, op1=ALU.add)
                            kb.act((rs, rs.t[:, 0:ng_]), (rs, rs.t[:, 0:ng_]), AF.Sqrt)
                            kb.op("dve", lambda hh, ng_=ng_: hh.reciprocal(out=rs.t[:, 0:ng_], in_=rs.t[:, 0:ng_]), reads=[rs], writes=[rs])
                            kb.tt("pool", (rs, rs.t[:, 0:ng_]), (rs, rs.t[:, 0:ng_]), (osum, osum.t[:, 0:ng_]), ALU.mult)
                            y_ = yo[cnt["yo"] % 2]
                            cnt["yo"] += 1
                            kb.stt("dve", (y_, y_.t[:, 0:ng_]), (rs, rs.t[:, 0:ng_]), V(hn), (sgT, sgT.t[:, c_lo * 128:c_lo * 128 + ng_]), ALU.mult, ALU.mult)
                            kb.ld("sp", (S["YT"], ytv[:, h, tgg:tgg + ng_]), (y_, y_.t[:, 0:ng_]), fill=True)

            for h in range(NH):
                for sg in range(5):
                    kb.ld("pool", (wh, wh.t[:, :, sg, :]),
                          (I["w_in"], I["w_in"].t.rearrange("(k p) n -> p k n", p=128)[:, :, sg * D + h * 128:sg * D + (h + 1) * 128]),
                          fill=(sg > 0))
                kb.ld("sp", V(hcT), (S["HCT"], hcv))
                for d in range(2):
                    kb.memset("pool", V(Sst[d]), 0.0)
                proj_v(hcT, LC, vctok, 0)
                for d in range(2):
                    zb_ = zTs[cnt["z"] % 2]
                    cnt["z"] += 1
                    proj_fm(hcT, LC, 2 + d, lambda pv_, zb_=zb_: kb.cp("dve", (zb_, zb_.t[:, 0:LC]), pv_))
                    gla_seg(h, d, NCT, zb_, None, vctok, 0, False, 0)
                for sgi in range(NSEG):
                    t0 = sgi * SEG
                    zT = zTs[cnt["z"] % 2]
                    cnt["z"] += 1
                    for half in range(SEG // HW):
                        tt0 = t0 + half * HW
                        ht = hts[cnt["ht"] % 2]
                        cnt["ht"] += 1
                        kb.ld("sp", V(ht), (S["HXT"], hxv[:, :, tt0:tt0 + HW]))
                        proj_fm(ht, HW, 0, lambda pv_, tt0=tt0: kb.act((qT, qT.t[:, tt0:tt0 + HW]), pv_, AF.Silu))
                        proj_fm(ht, HW, 2, lambda pv_, half=half, zT=zT: kb.cp("dve", (zT, zT.t[:, half * HW:(half + 1) * HW]), pv_))
                        proj_v(ht, HW, vtok, tt0 // 128)
                    gla_seg(h, 0, NCS, zT, (qT, qT.t[:, t0:t0 + SEG]), vtok, t0 // 128, True, t0)
                for sgi in range(NSEG - 1, -1, -1):
                    t0 = sgi * SEG
                    zT = zTs[cnt["z"] % 2]
                    sgT = sgTs[cnt["z"] % 2]
                    cnt["z"] += 1
                    for half in range(SEG // HW):
                        tt0 = t0 + half * HW
                        ht = hts[cnt["ht"] % 2]
                        cnt["ht"] += 1
                        kb.ld("sp", V(ht), (S["HXT"], hxv[:, :, tt0:tt0 + HW]))
                        proj_fm(ht, HW, 3, lambda pv_, half=half, zT=zT: kb.cp("dve", (zT, zT.t[:, half * HW:(half + 1) * HW]), pv_))
                        proj_fm(ht, HW, 1, lambda pv_, half=half, sgT=sgT: kb.act((sgT, sgT.t[:, half * HW:(half + 1) * HW]), pv_, AF.Silu))
                    gla_seg(h, 1, NCS, zT, (qT, qT.t[:, t0:t0 + SEG]), vtok, t0 // 128, True, t0, sgT)
            kb.barrier()
            kb.ctx = None

        with ExitStack() as sctx:
            kb.ctx = sctx
            wo = kb.sb("wo", [128, KD, D], BF16)
            kb.ld("pool", V(wo), (I["w_out"], I["w_out"].t.rearrange("(k p) n -> p k n", p=128)))
            GT = kb.sb("GTo", [128, D], F32)
            M = S["MOD1"]
            self.load_bcast(kb, V(GT), M.t[0, 2 * D:3 * D], M)
            yTs = [kb.sb("yT%d" % i, [128, NH, 512], BF16) for i in range(2)]
            xts = [kb.sb("xt%d" % i, [128, D], F32) for i in range(2)]
            ots = [kb.sb("ot%d" % i, [128, D], F32) for i in range(2)]
            pw = [kb.ps("pw%d" % i, [128, 512]) for i in range(4)]
            ytv = S["YT"].t.rearrange("(h p) t -> p h t", p=128)
            npw = 0
            for g in range((L + 511) // 512):
                t0 = g * 512
                n = min(512, L - t0)
                yT = yTs[g % 2]
                kb.ld("sp", (yT, yT.t[:, :, 0:n]), (S["YT"], ytv[:, :, t0:t0 + n]))
                for sub in range(n // 128):
                    i = (t0 // 128) + sub
                    xt = xts[i % 2]
                    kb.ld("sp", V(xt), (S["X2"], S["X2"].t[i * 128:(i + 1) * 128, :]))
                    ot = ots[i % 2]
                    for nb in range(D // 512):
                        p = pw[npw % 4]
                        npw += 1
                        for hh in range(NH):
                            kb.mm(V(p), (yT, yT.t[:, hh, sub * 128:(sub + 1) * 128]), (wo, wo.t[:, hh, nb * 512:(nb + 1) * 512]),
                                  start=(hh == 0), stop=(hh == NH - 1))
                        kb.tt("dve", (ot, ot.t[:, nb * 512:(nb + 1) * 512]), V(p), (GT, GT.t[:, nb * 512:(nb + 1) * 512]), ALU.mult)
                    kb.tt("pool", V(ot), V(ot), V(xt), ALU.add)
                    kb.ld("sp", (S["X3"], S["X3"].t[i * 128:(i + 1) * 128, :]), V(ot), fill=True)
            kb.barrier()
            kb.ctx = None


def make_in_maps(prog, inp):
    c = prog.cfg
    D, L, LC, KD, NE = prog.D, prog.L, prog.LC, prog.KD, prog.NE
    NH, CAP = c["NH"], c["CAP"]
    f = lambda a: np.ascontiguousarray(np.asarray(a, dtype=np.float32))
    gains = f(np.stack([inp["norm_mix"][0], inp["norm_mix"][1], inp["norm_ffn"][0], inp["norm_ffn"][1],
                        inp["norm_final"], inp["pool_scale"][0]], 0))
    wr = f(np.concatenate([inp["router_w_group"], inp["router_w_expert"]], -1))
    br = np.concatenate([inp["router_b_group"], inp["router_b_expert"]], -1)
    br = f(np.broadcast_to(br[:, None, :], (2, 128, br.shape[-1])))
    b_ada = f(np.broadcast_to(np.asarray(inp["b_ada"])[:, None, :], (2, 2, 6 * D)))
    ecap = f(np.broadcast_to((np.arange(NE) * CAP)[None, :], (128, NE)))
    tri = f(np.triu(np.ones((128, 128)), 1))
    lbl = f(np.asarray(inp["hgrn_lb_logits"]).reshape(2, 2, NH, 128).transpose(3, 0, 1, 2))
    hnorm = f(np.asarray(inp["hgrn_norm"]).reshape(128, 1))
    SEG = min(1024, L)
    m128 = np.ones((128, SEG), np.float32); m128[:, ::128] = 0
    m32 = np.ones((128, SEG), np.float32); m32[:, ::32] = 0
    st = np.arange(128)
    trimask = f(np.stack([(st[:, None] <= st[None, :]), (st[:, None] >= st[None, :])], 1))
    shared = dict(w_ada=f(inp["w_ada"]), b_ada=b_ada, gains=gains, pool_w=f(inp["pool_w"][0]),
                  bgrid=prog.Bgrid, rcgrid=prog.RCgrid, bseq=prog.Bseq, rcseq=prog.RCseq,
                  ident=np.eye(128, dtype=np.float32), wr=wr, br=br, ecap=ecap, tri=tri,
                  pcol=f(np.arange(128).reshape(128, 1)),
                  w_gu=f(inp["moe_w_gate_up"]), w_dn=f(inp["moe_w_down"]),
                  w_in=f(inp["hgrn_w_in"][0]), w_out=f(inp["hgrn_w_out"][0]), lbl=lbl, hnorm=hnorm,
                  mask128=m128, mask32=m32, trimask=trimask)
    maps = []
    for b in range(c["NCORES"]):
        cvec = np.stack([np.asarray(inp["c"][b]).reshape(KD, 128).T, np.asarray(inp["c_ctx"]).reshape(KD, 128).T], -1)
        m = dict(shared)
        m["x"] = f(inp["x"][b])
        m["ctx"] = f(inp["ctx"][b])
        m["cvec"] = f(cvec)
        maps.append(m)
    return maps


_CACHE = {}


def run(inp, cfg, debug=False, stop_after=None):
    prog = Prog(cfg, debug=debug, stop_after=stop_after)
    nc = prog.build()
    maps = make_in_maps(prog, inp)
    res = run_bass_kernel_spmd(nc, maps, core_ids=list(range(cfg["NCORES"])))
    return prog, res


def kernel(**inputs):
    cfg = FULL_CFG
    prog, res = run(inputs, cfg)
    out = np.stack([np.asarray(r["out"]) for r in res.results], 0)
    return out.astype(np.float32)
```

```python
import numpy as np
from contextlib import ExitStack
import concourse.bass as bass
import concourse.mybir as mybir
from concourse.bass_utils import run_bass_kernel_spmd

F32 = mybir.dt.float32
BF16 = mybir.dt.bfloat16
I32 = mybir.dt.int32
ALU = mybir.AluOpType
AF = mybir.ActivationFunctionType
AX = mybir.AxisListType
EPS = 1e-6

FULL_CFG = dict(D=2048, L=4096, GW=64, LC=256, NH=16, NG=4, EG=8, DE=1024, CAP=512, NS=8,
                WINS=(2, 4, 8, 16), NCORES=8)


class Buf:
    def __init__(self, t, name):
        self.t = t
        self.name = name
        self.w = None
        self.r = []
        self.dsem = None
        self.dcnt = 0

    def __getitem__(self, k):
        return self.t[k]

    def sub(self, key, name=None):
        return Buf(self.t[key], name or self.name + "_s")


class Eng:
    def __init__(self, name, sem):
        self.name = name
        self.sem = sem
        self.cnt = 0
        self.seen = {}
        self.prog = []


class KB:
    def __init__(self, nc, ctx, nsem=84):
        self.nc = nc
        self.gctx = ctx
        self.ctx = ctx
        self.E = {}
        for name in ("pe", "act", "dve", "pool", "sp"):
            s = ctx.enter_context(nc.semaphore("es_" + name))
            self.E[name] = Eng(name, s)
        self.pool_sems = [ctx.enter_context(nc.semaphore("ds%d" % i)) for i in range(nsem)]
        self.semval = {id(s): 0 for s in self.pool_sems}
        self.free_sems = list(self.pool_sems)
        self.phase_bufs = []
        self.n_ins = 0

    def sb(self, name, shape, dtype):
        self.uid = getattr(self, "uid", 0) + 1
        name = "s%d_%s" % (self.uid, name)
        t = self.ctx.enter_context(self.nc.sbuf_tensor(name, list(shape), dtype))
        return Buf(t, name)

    def ps(self, name, shape, dtype=F32):
        self.uid = getattr(self, "uid", 0) + 1
        name = "p%d_%s" % (self.uid, name)
        t = self.ctx.enter_context(self.nc.psum_tensor(name, list(shape), dtype))
        return Buf(t, name)

    def dram(self, name, shape, dtype, kind="Internal"):
        t = self.nc.dram_tensor(name, list(shape), dtype, kind=kind)
        return Buf(t.ap(), name)

    def _dsem(self, b):
        if b.dsem is None:
            b.dsem = self.free_sems.pop()
            b.dcnt = self.semval[id(b.dsem)]
            self.phase_bufs.append(b)
        return b.dsem

    def _filter(self, eng, deps, raw_same):
        out = {}
        for (sem, val, e) in deps:
            if e == eng.name and not raw_same:
                continue
            k = id(sem)
            if k not in out or out[k][1] < val:
                out[k] = (sem, val)
        res = []
        for k, (sem, val) in out.items():
            if eng.seen.get(k, 0) >= val:
                continue
            eng.seen[k] = val
            res.append((sem, val))
        return res

    def op(self, en, fn, reads=(), writes=(), raw_same=None):
        eng = self.E[en]
        if raw_same is None:
            raw_same = en != "pe"
        deps = []
        for b in reads:
            if b.w is not None:
                deps.append(b.w)
        for b in writes:
            if b.w is not None:
                deps.append(b.w)
            for r in b.r:
                if r[2] != en:
                    deps.append(r)
        waits = self._filter(eng, deps, raw_same)
        eng.cnt += 1
        rec = (eng.sem, eng.cnt, en)
        eng.prog.append((waits, fn, (eng.sem, 1)))
        for b in reads:
            b.r.append(rec)
        for b in writes:
            b.w = rec
            b.r = []
        self.n_ins += 1
        return rec

    def dma(self, qn, fn, reads=(), writes=(), fill=False):
        eng = self.E[qn]
        deps = []
        for b in reads:
            if b.w is not None:
                deps.append(b.w)
        for b in writes:
            if b.w is not None and not (fill and b.w[2] == "dma"):
                deps.append(b.w)
            deps.extend(b.r)
        waits = self._filter(eng, deps, True)
        tgt = writes[0]
        sem = self._dsem(tgt)
        tgt.dcnt += 16
        self.semval[id(sem)] = tgt.dcnt
        rec = (sem, tgt.dcnt, "dma")
        eng.prog.append((waits, fn, (sem, 16)))
        for b in reads:
            b.r.append(rec)
        for b in writes:
            b.w = rec
            if not fill:
                b.r = []
        self.n_ins += 1
        return rec

    def pool_reg(self, value):
        if getattr(self, "_preg", None) is None:
            self._preg = self.nc.alloc_register(mybir.EngineType.Pool, "bcreg")
        reg = self._preg
        self.E["pool"].prog.append(([], lambda h: h.reg_mov(reg, value), None))
        return reg

    def barrier(self):
        targets = [(e.sem, e.cnt) for e in self.E.values() if e.cnt > 0]
        for b in self.phase_bufs:
            targets.append((b.dsem, b.dcnt))
        for eng in self.E.values():
            waits = []
            for (sem, val) in targets:
                if sem is eng.sem:
                    continue
                if eng.seen.get(id(sem), 0) >= val:
                    continue
                eng.seen[id(sem)] = val
                waits.append((sem, val))
            eng.prog.append((waits, None, None))

    def end_phase(self):
        self.barrier()
        self.emit()
        for b in self.phase_bufs:
            self.free_sems.append(b.dsem)
            b.dsem = None
        self.phase_bufs = []

    def emit(self):
        nc = self.nc
        with nc.Block() as block:
            def run(eng):
                def body(h):
                    for waits, fn, inc in eng.prog:
                        for (sem, val) in waits:
                            h.wait_ge(sem, val)
                        if fn is not None:
                            ins = fn(h)
                            if inc is not None:
                                ins.then_inc(inc[0], inc[1])
                    eng.prog = []
                return body
            block.tensor(run(self.E["pe"]))
            block.scalar(run(self.E["act"]))
            block.vector(run(self.E["dve"]))
            block.gpsimd(run(self.E["pool"]))
            block.sync(run(self.E["sp"]))

    @staticmethod
    def _s(x):
        return x[1] if isinstance(x, tuple) else x

    @staticmethod
    def _bufs(*xs):
        return [x[0] for x in xs if isinstance(x, tuple)]

    def mm(self, out, lhsT, rhs, start=True, stop=True, tr=False):
        self.op("pe", lambda h: h.matmul(out[1], lhsT=lhsT[1], rhs=rhs[1], start=start, stop=stop,
                                          is_transpose=(True if tr else None)),
                reads=[lhsT[0], rhs[0]], writes=[out[0]])

    def tt(self, en, out, in0, in1, op):
        self.op(en, lambda h: h.tensor_tensor(out=out[1], in0=in0[1], in1=in1[1], op=op),
                reads=[in0[0], in1[0]], writes=[out[0]])

    def ts(self, en, out, in0, s1, s2=None, op0=ALU.mult, op1=None, accum=None):
        kw = {}
        if op1 is not None:
            kw["op1"] = op1
        if accum is not None:
            kw["accum_out"] = accum[1]
        self.op(en, lambda h: h.tensor_scalar(out=out[1], in0=in0[1], scalar1=self._s(s1), scalar2=self._s(s2),
                                               op0=op0, **kw),
                reads=[in0[0]] + self._bufs(s1, s2), writes=[out[0]] + self._bufs(accum))

    def stt(self, en, out, in0, scalar, in1, op0, op1):
        self.op(en, lambda h: h.scalar_tensor_tensor(out=out[1], in0=in0[1], scalar=self._s(scalar), in1=in1[1],
                                                      op0=op0, op1=op1),
                reads=[in0[0], in1[0]] + self._bufs(scalar), writes=[out[0]])

    def act(self, out, in_, func, bias=None, scale=None, accum=None):
        kw = {}
        if bias is not None:
            kw["bias"] = self._s(bias)
        if scale is not None:
            kw["scale"] = self._s(scale)
        if accum is not None:
            kw["accum_out"] = accum[1]
        self.op("act", lambda h: h.activation(out=out[1], in_=in_[1], func=func, **kw),
                reads=[in_[0]] + self._bufs(bias, scale), writes=[out[0]] + self._bufs(accum))

    def cp(self, en, out, in_):
        if en == "act":
            self.op(en, lambda h: h.copy(out=out[1], in_=in_[1]), reads=[in_[0]], writes=[out[0]])
        else:
            self.op(en, lambda h: h.tensor_copy(out=out[1], in_=in_[1]), reads=[in_[0]], writes=[out[0]])

    def red(self, en, out, in_, op, axis=AX.X):
        self.op(en, lambda h: h.tensor_reduce(out=out[1], in_=in_[1], axis=axis, op=op),
                reads=[in_[0]], writes=[out[0]])

    def memset(self, en, out, val):
        self.op(en, lambda h: h.memset(out[1], val), reads=[], writes=[out[0]])

    def ld(self, qn, out, in_, fill=False):
        self.dma(qn, lambda h: h.dma_start(out=out[1], in_=in_[1]), reads=[in_[0]], writes=[out[0]], fill=fill)


def V(b, key=None):
    return (b, b.t[key] if key is not None else b.t[:])


def _win(k):
    return k // 2, k - k // 2 - 1


def pool_consts_grid(cfg):
    GW, L, WINS = cfg["GW"], cfg["L"], cfg["WINS"]
    RPT = 128 // GW
    NT = L // 128
    rows = L // GW
    offs = []
    blocks = []
    bidx = {}
    cls_rows = []
    cls_of = {}
    diag_idx = {}
    rs = np.arange(128) // GW
    cs = np.arange(128) % GW
    for j, k in enumerate(WINS):
        lo, hi = _win(k)
        dlo = (-lo) // RPT
        dhi = (RPT - 1 + hi) // RPT
        offs.append(list(range(dlo, dhi + 1)))
        for d in range(dlo, dhi + 1):
            rd = (RPT * d + rs[:, None]) - rs[None, :]
            cd = cs[:, None] - cs[None, :]
            W = ((rd >= -lo) & (rd <= hi) & (cd >= -lo) & (cd <= hi)).astype(np.float32)
            if d != 0:
                bidx[(j, d)] = len(blocks)
                blocks.append(W)
            else:
                W0 = W
        for i in range(NT):
            r = RPT * i + rs
            cr = np.minimum(r + hi, rows - 1) - np.maximum(r - lo, 0) + 1
            cc = np.minimum(cs + hi, GW - 1) - np.maximum(cs - lo, 0) + 1
            cnt = (cr * cc).astype(np.float32)
            key = (j, tuple(cnt.tolist()))
            if key not in diag_idx:
                diag_idx[key] = (len(blocks), len(cls_rows))
                blocks.append(W0 - np.diag(cnt))
                cls_rows.append(1.0 / cnt)
            cls_of[(j, i)] = diag_idx[key]
    B = np.stack(blocks, 1).astype(np.float32)
    RC = np.stack(cls_rows, 0).astype(np.float32)
    RC = np.broadcast_to(RC[None], (128,) + RC.shape).copy()
    return offs, bidx, cls_of, B, RC


def pool_consts_seq(cfg):
    LC, WINS = cfg["LC"], cfg["WINS"]
    NCT = LC // 128
    pos = np.arange(LC)
    Bs = np.zeros((128, len(WINS), NCT, NCT, 128), np.float32)
    RC = np.zeros((len(WINS), NCT, 128), np.float32)
    for j, k in enumerate(WINS):
        lo, hi = _win(k)
        d = pos[:, None] - pos[None, :]
        W = ((d >= -lo) & (d <= hi)).astype(np.float32)
        cnt = (np.minimum(pos + hi, LC - 1) - np.maximum(pos - lo, 0) + 1).astype(np.float32)
        W = W - np.diag(cnt)
        for a in range(NCT):
            for b in range(NCT):
                Bs[:, j, a, b, :] = W[a * 128:(a + 1) * 128, b * 128:(b + 1) * 128]
        RC[j] = (1.0 / cnt).reshape(NCT, 128)
    RC = np.broadcast_to(RC[None], (128,) + RC.shape).copy()
    return Bs, RC


class Prog:
    def __init__(self, cfg, debug=False, stop_after=None):
        self.cfg = cfg
        self.debug = debug
        self.stop_after = stop_after
        c = cfg
        self.D, self.L, self.LC = c["D"], c["L"], c["LC"]
        self.KD = self.D // 128
        self.NT = self.L // 128
        self.NCT = self.LC // 128
        self.NE = c["NG"] * c["EG"]
        self.NR = c["NG"] + self.NE
        self.offs, self.bidx, self.cls_of, self.Bgrid, self.RCgrid = pool_consts_grid(cfg)
        self.Bseq, self.RCseq = pool_consts_seq(cfg)

    def build(self):
        c = self.cfg
        D, L, LC, KD, NE = self.D, self.L, self.LC, self.KD, self.NE
        nc = bass.Bass("TRN2", target_bir_lowering=False)
        self.nc = nc
        with ExitStack() as gctx:
            kb = KB(nc, gctx)
            self.kb = kb
            I = {}

            def inp(name, shape, dt=F32):
                I[name] = kb.dram(name, shape, dt, kind="ExternalInput")
            inp("x", [L, D]); inp("ctx", [LC, D]); inp("cvec", [128, KD, 2])
            inp("w_ada", [2, D, 6 * D]); inp("b_ada", [2, 2, 6 * D])
            inp("gains", [6, D])
            inp("pool_w", [4, D // 4, D // 4])
            inp("bgrid", list(self.Bgrid.shape)); inp("rcgrid", list(self.RCgrid.shape))
            inp("bseq", list(self.Bseq.shape)); inp("rcseq", list(self.RCseq.shape))
            inp("ident", [128, 128])
            inp("wr", [2, D, self.NR]); inp("br", [2, 128, self.NR])
            inp("ecap", [128, NE]); inp("tri", [128, 128]); inp("pcol", [128, 1])
            inp("w_gu", [2, NE, D, 2 * c["DE"]]); inp("w_dn", [2, NE, c["DE"], D])
            inp("w_in", [D, 5 * D]); inp("w_out", [D, D]); inp("lbl", [128, 2, 2, c["NH"]]); inp("hnorm", [128, 1])
            self.SEG = min(1024, L)
            inp("mask128", [128, self.SEG]); inp("mask32", [128, self.SEG]); inp("trimask", [128, 2, 128])
            self.I = I
            okind = "ExternalOutput" if self.debug else "Internal"
            S = {}
            S["MOD0"] = kb.dram("MOD0", [2, 6 * D], F32, kind=okind)
            S["MOD1"] = kb.dram("MOD1", [2, 6 * D], F32, kind=okind)
            S["X1"] = kb.dram("X1", [L, D], F32, kind=okind)
            S["C1"] = kb.dram("C1", [LC, D], F32, kind=okind)
            S["X2"] = kb.dram("X2", [L, D], F32, kind=okind)
            S["C2"] = kb.dram("C2", [LC, D], F32, kind=okind)
            S["X3"] = kb.dram("X3", [L, D], F32, kind=okind)
            NROWS = NE * c["CAP"] + c["NS"] * 128
            S["XS"] = kb.dram("XS", [NROWS, D], BF16)
            S["YS"] = kb.dram("YS", [NROWS, D], F32)
            S["FB"] = kb.dram("FB", [L + LC, D], BF16)
            S["HXT"] = kb.dram("HXT", [D, L], BF16)
            S["HCT"] = kb.dram("HCT", [D, LC], BF16)
            S["YT"] = kb.dram("YT", [D, L], BF16)
            S["OUT"] = kb.dram("out", [L, D], F32, kind="ExternalOutput")
            self.S = S

            stages = [("ada", self.phase_ada),
                      ("mix0", self.phase_pool),
                      ("moe0", lambda: self.phase_moe(0)),
                      ("hgrn", self.phase_hgrn),
                      ("moe1", lambda: self.phase_moe(1))]
            for name, fn in stages:
                with ExitStack() as pctx:
                    kb.ctx = pctx
                    fn()
                    kb.end_phase()
                kb.ctx = gctx
                if self.stop_after == name:
                    break
        return nc

    def load_bcast(self, kb, dst, src_row_ap, src_buf, qn="sp"):
        kb.dma(qn, lambda h: h.dma_start(out=dst[1], in_=src_row_ap.partition_broadcast(128)),
               reads=[src_buf], writes=[dst[0]])

    def rstd_of(self, kb, rstd, ssq, n):
        kb.ts("dve", V(rstd), V(ssq), 1.0 / n, EPS, op0=ALU.mult, op1=ALU.add)
        kb.act(V(rstd), V(rstd), AF.Sqrt)
        kb.op("dve", lambda h: h.reciprocal(out=rstd.t[:], in_=rstd.t[:]), reads=[rstd], writes=[rstd])

    def rms_mod(self, kb, xt, G, SH, t1, out, ssq, rstd, junk, D):
        kb.act(V(junk), V(xt), AF.Square, accum=V(ssq))
        self.rstd_of(kb, rstd, ssq, D)
        kb.stt("dve", V(t1), V(xt), V(rstd), V(G), ALU.mult, ALU.mult)
        kb.tt("pool", V(out), V(t1), V(SH), ALU.add)

    def phase_ada(self):
        kb, I, S = self.kb, self.I, self.S
        D, KD = self.D, self.KD
        N6 = 6 * D
        NB = N6 // 512
        cv = kb.sb("cv", [128, KD, 2], F32)
        sv = kb.sb("sv", [128, KD, 2], BF16)
        kb.ld("sp", V(cv), V(I["cvec"]))
        kb.act(V(sv), V(cv), AF.Silu)
        wts = [kb.sb("wa%d" % i, [128, KD, 512], BF16) for i in range(2)]
        pss = [kb.ps("pa%d" % i, [128, 512]) for i in range(2)]
        bada = kb.sb("bada", [2, N6], F32)
        mrow = kb.sb("mrow", [2, N6], F32)
        for i in range(2):
            kb.ld("sp", V(bada), (I["b_ada"], I["b_ada"].t[i]))
            wsrc = I["w_ada"].t[i].rearrange("(k p) n -> p k n", p=128)
            for nb in range(NB):
                wt = wts[nb % 2]
                ps = pss[nb % 2]
                kb.ld("pool", V(wt), (I["w_ada"], wsrc[:, :, nb * 512:(nb + 1) * 512]))
                for k in range(KD):
                    kb.mm((ps, ps.t[0:2, :]), (sv, sv.t[:, k, :]), (wt, wt.t[:, k, :]), start=(k == 0), stop=(k == KD - 1))
                kb.tt("dve", (mrow, mrow.t[:, nb * 512:(nb + 1) * 512]), (ps, ps.t[0:2, :]),
                      (bada, bada.t[:, nb * 512:(nb + 1) * 512]), ALU.add)
            kb.ld("sp", V(S["MOD%d" % i]), V(mrow))

    def mod_vecs(self, kb, layer, cond, which, gain_row, tag):
        D = self.D
        M = self.S["MOD%d" % layer]
        base = 3 * which * D
        G = kb.sb("G" + tag, [128, D], F32)
        SH = kb.sb("SH" + tag, [128, D], F32)
        GT = kb.sb("GT" + tag, [128, D], F32)
        gn = kb.sb("gn" + tag, [128, D], F32)
        self.load_bcast(kb, V(SH), M.t[cond, base:base + D], M)
        self.load_bcast(kb, V(G), M.t[cond, base + D:base + 2 * D], M)
        self.load_bcast(kb, V(GT), M.t[cond, base + 2 * D:base + 3 * D], M)
        self.load_bcast(kb, V(gn), self.I["gains"].t[gain_row, :], self.I["gains"])
        kb.stt("dve", V(G), V(G), 1.0, V(gn), ALU.add, ALU.mult)
        return G, SH, GT, gn

    def phase_pool(self):
        kb, I, S = self.kb, self.I, self.S
        c = self.cfg
        D, KD, NT, NCT = self.D, self.KD, self.NT, self.NCT
        PG = D // 4
        PGC = PG // 128
        WINS = c["WINS"]
        NBLK = self.Bgrid.shape[1]
        NCLS = self.RCgrid.shape[1]
        bg = kb.sb("bg", [128, NBLK, 128], BF16)
        kb.ld("pool", V(bg), V(I["bgrid"]))
        rc = kb.sb("rc", [128, NCLS, 128], F32)
        kb.ld("sp", V(rc), V(I["rcgrid"]))
        bs = kb.sb("bs", [128, 4, NCT, NCT, 128], BF16)
        kb.ld("pool", V(bs), V(I["bseq"]))
        rcs = kb.sb("rcs", [128, 4, NCT, 128], F32)
        kb.ld("sp", V(rcs), V(I["rcseq"]))
        wp = kb.sb("wp", [128, 4, PGC, PG], BF16)
        kb.ld("pool", V(wp), (I["pool_w"], I["pool_w"].t.rearrange("j (c p) n -> p j c n", p=128)))
        ssq = kb.sb("ssq", [128, 1], F32)
        rstd = kb.sb("rstd", [128, 1], F32)
        junk = kb.sb("junk", [128, D], BF16)
        t1 = kb.sb("t1", [128, D], F32)
        xts = [kb.sb("xt%d" % i, [128, D], F32) for i in range(2)]
        xrs = [kb.sb("xr%d" % i, [128, D], F32) for i in range(2)]
        dT = kb.sb("dT", [128, KD, 128], BF16)
        yts = [kb.sb("yt%d" % i, [128, D], F32) for i in range(2)]
        pds = [kb.ps("pd%d" % i, [128, 512]) for i in range(2)]
        pys = [kb.ps("py%d" % i, [128, 512]) for i in range(2)]
        RING = 10
        hbig = kb.sb("hring", [128, RING, D], BF16)
        hring = [hbig.sub((slice(None), r, slice(None)), "h%d" % r) for r in range(RING)]

        for (cond, src, dst, ntile) in ((0, I["x"], S["X1"], NT), (1, I["ctx"], S["C1"], NCT)):
            tag = "m0%d" % cond
            with ExitStack() as sctx:
                old = kb.ctx
                kb.ctx = sctx
                G, SH, GT, gn = self.mod_vecs(kb, 0, cond, 0, 0, tag)
                self.load_bcast(kb, V(gn), I["gains"].t[5, :], I["gains"])
                kb.tt("dve", V(GT), V(GT), V(gn), ALU.mult)
                if cond == 0:
                    lead = max(max(o) for o in self.offs)
                else:
                    lead = NCT - 1
                npd = 0

                def make_h(i):
                    xt = xts[i % 2]
                    kb.ld("sp", V(xt), (src, src.t[i * 128:(i + 1) * 128, :]))
                    self.rms_mod(kb, xt, G, SH, t1, hring[i % RING], ssq, rstd, junk, D)

                for i in range(min(lead, ntile)):
                    make_h(i)
                for i in range(ntile):
                    if i + lead < ntile:
                        make_h(i + lead)
                    xr = xrs[i % 2]
                    kb.ld("sp", V(xr), (src, src.t[i * 128:(i + 1) * 128, :]))
                    yt = yts[i % 2]
                    for j in range(4):
                        if cond == 0:
                            nb = []
                            for d in self.offs[j]:
                                s = i + d
                                if s < 0 or s >= ntile:
                                    continue
                                if d == 0:
                                    blk = self.cls_of[(j, i)][0]
                                else:
                                    blk = self.bidx[(j, d)]
                                nb.append((s, (bg, bg.t[:, blk, :])))
                            rcap = (rc, rc.t[:, self.cls_of[(j, i)][1], :])
                        else:
                            nb = [(s, (bs, bs.t[:, j, s, i, :])) for s in range(ntile)]
                            rcap = (rcs, rcs.t[:, j, i, :])
                        pd = pds[npd % 2]
                        npd += 1
                        for cc in range(PGC):
                            ch = j * PG + cc * 128
                            for n, (s, bap) in enumerate(nb):
                                hb = hring[s % RING]
                                kb.mm((pd, pd.t[:, cc * 128:(cc + 1) * 128]), (hb, hb.t[:, ch:ch + 128]), bap,
                                      start=(n == 0), stop=(n == len(nb) - 1))
                        for cc in range(PGC):
                            kb.tt("dve", (dT, dT.t[:, j * PGC + cc, :]), (pd, pd.t[:, cc * 128:(cc + 1) * 128]), rcap, ALU.mult)
                        py = pys[j % 2]
                        for cc in range(PGC):
                            kb.mm((py, py.t[:, 0:PG]), (dT, dT.t[:, j * PGC + cc, :]), (wp, wp.t[:, j, cc, :]),
                                  start=(cc == 0), stop=(cc == PGC - 1))
                        kb.tt("dve", (yt, yt.t[:, j * PG:(j + 1) * PG]), (py, py.t[:, 0:PG]), (GT, GT.t[:, j * PG:(j + 1) * PG]), ALU.mult)
                    kb.tt("pool", V(yt), V(yt), V(xr), ALU.add)
                    kb.ld("sp", (dst, dst.t[i * 128:(i + 1) * 128, :]), V(yt), fill=True)
                kb.barrier()
                kb.ctx = old

    def phase_moe(self, layer):
        kb, I, S = self.kb, self.I, self.S
        c = self.cfg
        D, KD, NT, NCT, NE, NR = self.D, self.KD, self.NT, self.NCT, self.NE, self.NR
        NG, EG, DE, CAP = c["NG"], c["EG"], c["DE"], c["CAP"]
        DEC = DE // 128
        last = layer == 1
        if layer == 0:
            tiles = [(0, S["X1"], S["X2"], i) for i in range(NT)] + [(1, S["C1"], S["C2"], i) for i in range(NCT)]
        else:
            tiles = [(0, S["X3"], S["OUT"], i) for i in range(NT)]
        NTM = len(tiles)
        NS = c["NS"]
        SP0 = NE * CAP
        NROWS = SP0 + NS * 128
        breg = kb.pool_reg(NROWS - 1)
        SL = kb.sb("SL", [128, NTM, 2], I32)
        SL2 = kb.sb("SL2", [128, NTM, 2], I32)
        WT = kb.sb("WT", [128, NTM, 2], F32)
        RK = kb.sb("RK", [128, NTM, 2], F32)
        MS = kb.sb("MS", [128, NTM, 2], F32)
        BASE = kb.sb("BASE", [128, NE], F32)
        NBLK = kb.sb("NBLK", [128, NE], F32)
        ecap = kb.sb("ecap", [128, NE], F32)
        kb.ld("sp", V(ecap), V(I["ecap"]))

        with ExitStack() as sctx:
            kb.ctx = sctx
            mods = {0: self.mod_vecs(kb, layer, 0, 1, 2 + layer, "f0")}
            if layer == 0:
                mods[1] = self.mod_vecs(kb, layer, 1, 1, 2 + layer, "f1")
            ident = kb.sb("identf", [128, 128], F32)
            kb.ld("sp", V(ident), V(I["ident"]))
            wr = kb.sb("wr", [128, KD, NR], F32)
            kb.ld("sp", V(wr), (I["wr"], I["wr"].t[layer].rearrange("(k p) n -> p k n", p=128)))
            br = kb.sb("br", [128, NR], F32)
            kb.ld("sp", V(br), (I["br"], I["br"].t[layer]))
            tri = kb.sb("tri", [128, 128], F32)
            kb.ld("sp", V(tri), V(I["tri"]))
            ones = kb.sb("ones", [128, 128], F32)
            kb.memset("dve", V(ones), 1.0)
            Aall = kb.sb("Aall", [128, NTM, NE], F32)
            ssq = kb.sb("ssq", [128, 1], F32)
            rstd = kb.sb("rstd", [128, 1], F32)
            junk = kb.sb("junk", [128, D], BF16)
            t1 = kb.sb("t1", [128, D], F32)
            xts = [kb.sb("xt%d" % i, [128, D], F32) for i in range(2)]
            ff = kb.sb("ff", [128, D], F32)
            fbs = [kb.sb("fb%d" % i, [128, D], BF16) for i in range(2)]
            fT = kb.sb("fT", [128, KD, 128], F32)
            ptr = [kb.ps("ptr%d" % i, [128, 512]) for i in range(2)]
            plg = kb.ps("plg", [128, 512])
            prk = kb.ps("prk", [128, 512])
            lg = kb.sb("lg", [128, NR], F32)
            sm = kb.sb("sm", [128, 64], F32)
            gmask = kb.sb("gmask", [128, NG], F32)
            gexp = kb.sb("gexp", [128, NG], F32)
            lsel = kb.sb("lsel", [128, EG], F32)
            l2 = kb.sb("l2", [128, EG], F32)
            mk = [kb.sb("mk%d" % i, [128, EG], F32) for i in range(2)]
            oh = [kb.sb("oh%d" % i, [128, NE], F32) for i in range(2)]
            rk = kb.sb("rk", [128, NE], F32)
            prod = kb.sb("prod", [128, NE], F32)
            slf = kb.sb("slf", [128, 2], F32)

            def col(i):
                return (sm, sm.t[:, i:i + 1])

            for ti, (cond, src, dst, i) in enumerate(tiles):
                G, SH, GT, gn = mods[cond]
                xt = xts[ti % 2]
                kb.ld("sp", V(xt), (src, src.t[i * 128:(i + 1) * 128, :]))
                self.rms_mod(kb, xt, G, SH, t1, ff, ssq, rstd, junk, D)
                fb = fbs[ti % 2]
                kb.cp("act", V(fb), V(ff))
                for k in range(KD):
                    p = ptr[(k // 4) % 2]
                    kb.mm((p, p.t[:, (k % 4) * 128:(k % 4 + 1) * 128]), (ff, ff.t[:, k * 128:(k + 1) * 128]), V(ident), tr=True)
                    if k % 4 == 3 or k == KD - 1:
                        k0 = (k // 4) * 4
                        n = k - k0 + 1
                        kb.cp("dve", (fT, fT.t[:, k0:k0 + n, :]), (p, p.t[:, 0:n * 128].rearrange("p (a b) -> p a b", b=128)))
                for k in range(KD):
                    kb.mm((plg, plg.t[:, 0:NR]), (fT, fT.t[:, k, :]), (wr, wr.t[:, k, :]), start=(k == 0), stop=(k == KD - 1))
                kb.tt("dve", V(lg), (plg, plg.t[:, 0:NR]), V(br), ALU.add)
                kb.red("dve", col(0), (lg, lg.t[:, 0:NG]), ALU.max)
                kb.ts("dve", V(gmask), (lg, lg.t[:, 0:NG]), col(0), None, op0=ALU.is_equal)
                kb.ts("dve", col(1), col(0), -1.0, None, op0=ALU.mult)
                kb.act(V(gexp), (lg, lg.t[:, 0:NG]), AF.Exp, bias=col(1), scale=1.0)
                kb.red("dve", col(2), V(gexp), ALU.add)
                kb.op("dve", lambda h: h.reciprocal(out=sm.t[:, 3:4], in_=sm.t[:, 2:3]), reads=[sm], writes=[sm])
                for g in range(NG):
                    le = (lg, lg.t[:, NG + g * EG:NG + (g + 1) * EG])
                    if g == 0:
                        kb.ts("dve", V(lsel), le, (gmask, gmask.t[:, 0:1]), None, op0=ALU.mult)
                    else:
                        kb.stt("dve", V(lsel), le, (gmask, gmask.t[:, g:g + 1]), V(lsel), ALU.mult, ALU.add)
                kb.red("dve", col(4), V(lsel), ALU.max)
                kb.ts("dve", V(mk[0]), V(lsel), col(4), None, op0=ALU.is_equal)
                kb.stt("dve", V(l2), V(mk[0]), -1e30, V(lsel), ALU.mult, ALU.add)
                kb.red("dve", col(5), V(l2), ALU.max)
                kb.ts("dve", V(mk[1]), V(l2), col(5), None, op0=ALU.is_equal)
                kb.tt("dve", col(6), col(5), col(4), ALU.subtract)
                kb.act(col(7), col(6), AF.Exp)
                kb.ts("dve", col(8), col(7), 1.0, None, op0=ALU.add)
                kb.op("dve", lambda h: h.reciprocal(out=sm.t[:, 9:10], in_=sm.t[:, 8:9]), reads=[sm], writes=[sm])
                kb.tt("dve", (WT, WT.t[:, ti, 0:1]), col(3), col(9), ALU.mult)
                kb.tt("dve", (WT, WT.t[:, ti, 1:2]), (WT, WT.t[:, ti, 0:1]), col(7), ALU.mult)
                for kk in range(2):
                    for g in range(NG):
                        kb.ts("dve", (oh[kk], oh[kk].t[:, g * EG:(g + 1) * EG]), V(mk[kk]), (gmask, gmask.t[:, g:g + 1]), None, op0=ALU.mult)
                kb.tt("dve", (Aall, Aall.t[:, ti, :]), V(oh[0]), V(oh[1]), ALU.add)
                for tj in range(ti):
                    kb.mm((prk, prk.t[:, 0:NE]), V(ones), (Aall, Aall.t[:, tj, :]), start=(tj == 0), stop=False)
                kb.mm((prk, prk.t[:, 0:NE]), V(tri), (Aall, Aall.t[:, ti, :]), start=(ti == 0), stop=True)
                kb.cp("dve", V(rk), (prk, prk.t[:, 0:NE]))
                for kk in range(2):
                    kb.tt("dve", V(prod), V(oh[kk]), V(rk), ALU.mult)
                    kb.red("dve", col(10 + kk), V(prod), ALU.add)
                    kb.tt("dve", V(prod), V(oh[kk]), V(ecap), ALU.mult)
                    kb.red("dve", col(12 + kk), V(prod), ALU.add)
                    kb.ts("dve", col(14 + kk), col(10 + kk), float(CAP), 1e9, op0=ALU.is_ge, op1=ALU.mult)
                    kb.cp("dve", (RK, RK.t[:, ti, kk:kk + 1]), col(10 + kk))
                    kb.tt("dve", (MS, MS.t[:, ti, kk:kk + 1]), col(12 + kk), col(10 + kk), ALU.add)
                    kb.tt("dve", (slf, slf.t[:, kk:kk + 1]), (MS, MS.t[:, ti, kk:kk + 1]), col(14 + kk), ALU.add)
                kb.ts("dve", V(slf), V(slf), 2.0e9, None, op0=ALU.min)
                kb.cp("dve", (SL, SL.t[:, ti, :]), V(slf))
                kb.ld("sp", (S["FB"], S["FB"].t[ti * 128:(ti + 1) * 128, :]), V(fb), fill=True)
                for kk in range(2):
                    kb.dma("pool", lambda h, kk=kk, fb=fb, ti=ti: h.indirect_dma_start(
                        out=S["XS"].t[:, :], out_offset=bass.IndirectOffsetOnAxis(ap=SL.t[:, ti, kk:kk + 1], axis=0),
                        in_=fb.t[:, :], in_offset=None, bounds_check=breg, oob_is_err=False),
                        reads=[fb, SL], writes=[S["XS"]], fill=True)
            for tj in range(NTM):
                kb.mm((prk, prk.t[:, 0:NE]), V(ones), (Aall, Aall.t[:, tj, :]), start=(tj == 0), stop=(tj == NTM - 1))
            kb.ts("dve", V(rk), (prk, prk.t[:, 0:NE]), -float(CAP), 0.0, op0=ALU.add, op1=ALU.max)
            kb.ts("dve", V(NBLK), V(rk), 0.0, None, op0=ALU.is_gt)
            for j in range(1, NS):
                kb.stt("dve", V(NBLK), V(rk), float(128 * j), V(NBLK), ALU.is_gt, ALU.add)
            kb.op("dve", lambda h: h.tensor_tensor_scan(out=BASE.t[:], data0=ones.t[:, 0:NE], data1=NBLK.t[:], initial=0.0,
                                                          op0=ALU.mult, op1=ALU.add), reads=[ones, NBLK], writes=[BASE])
            kb.tt("dve", V(BASE), V(BASE), V(NBLK), ALU.subtract)
            for ti in range(NTM):
                fb = fbs[ti % 2]
                kb.ld("sp", V(fb), (S["FB"], S["FB"].t[ti * 128:(ti + 1) * 128, :]))
                for kk in range(2):
                    rkc = (RK, RK.t[:, ti, kk:kk + 1])
                    msc = (MS, MS.t[:, ti, kk:kk + 1])
                    kb.tt("dve", col(19), msc, rkc, ALU.subtract)
                    kb.ts("dve", V(oh[0]), V(ecap), col(19), None, op0=ALU.is_equal)
                    kb.tt("dve", V(prod), V(oh[0]), V(BASE), ALU.mult)
                    kb.red("dve", col(20), V(prod), ALU.add)
                    kb.ts("dve", col(21), rkc, float(CAP), None, op0=ALU.is_ge)
                    kb.stt("dve", col(22), col(20), 128.0, rkc, ALU.mult, ALU.add)
                    kb.ts("dve", col(22), col(22), float(SP0 - CAP), None, op0=ALU.add)
                    kb.ts("dve", col(23), col(22), float(NROWS), None, op0=ALU.is_lt)
                    kb.tt("dve", col(23), col(23), col(21), ALU.mult)
                    kb.ts("dve", col(24), col(23), -1e9, 1e9, op0=ALU.mult, op1=ALU.add)
                    kb.tt("dve", col(25), col(22), col(23), ALU.mult)
                    kb.tt("dve", (slf, slf.t[:, kk:kk + 1]), col(25), col(24), ALU.add)
                    kb.ts("dve", col(26), col(21), -1.0, 1.0, op0=ALU.mult, op1=ALU.add)
                    kb.tt("dve", col(27), msc, col(26), ALU.mult)
                    kb.tt("dve", col(27), col(27), col(25), ALU.add)
                    kb.tt("dve", col(28), col(21), col(23), ALU.subtract)
                    kb.stt("dve", (sm, sm.t[:, 30 + kk:31 + kk]), col(28), 1e9, col(27), ALU.mult, ALU.add)
                kb.cp("dve", (SL2, SL2.t[:, ti, :]), V(slf))
                kb.cp("dve", (SL, SL.t[:, ti, :]), (sm, sm.t[:, 30:32]))
                for kk in range(2):
                    kb.dma("pool", lambda h, kk=kk, fb=fb, ti=ti: h.indirect_dma_start(
                        out=S["XS"].t[:, :], out_offset=bass.IndirectOffsetOnAxis(ap=SL2.t[:, ti, kk:kk + 1], axis=0),
                        in_=fb.t[:, :], in_offset=None, bounds_check=breg, oob_is_err=False),
                        reads=[fb, SL2], writes=[S["XS"]], fill=True)
            kb.barrier()
            kb.ctx = old = None

        with ExitStack() as sctx:
            kb.ctx = sctx
            NBK = CAP // 128
            NHB = (CAP + 511) // 512
            identb = kb.sb("identb", [128, 128], BF16)
            kb.ld("pool", V(identb), V(I["ident"]))
            xsT = [kb.sb("xsT%d" % i, [128, KD, CAP], BF16) for i in range(2)]
            HT = [kb.sb("HT%d" % i, [128, DEC, CAP], BF16) for i in range(2)]
            xrows = [kb.sb("xrow%d" % i, [128, D], BF16) for i in range(2)]
            wgs = [kb.sb("wg%d" % i, [128, KD, 2, 128], BF16) for i in range(3)]
            wds = [kb.sb("wd%d" % i, [128, DEC, 512], BF16) for i in range(3)]
            sas = [kb.sb("sa%d" % i, [128, 512], F32) for i in range(2)]
            yos = [kb.sb("yo%d" % i, [128, 512], F32) for i in range(3)]
            ptb = [kb.ps("ptb%d" % i, [128, 512], BF16) for i in range(2)]
            pab = [kb.ps("pab%d" % i, [128, 512]) for i in range(4)]
            pyy = [kb.ps("pyy%d" % i, [128, 512]) for i in range(2)]
            nwg = nwd = nsa = nyo = npy = npab = nxr = 0
            for e in range(NE):
                xT = xsT[e % 2]
                hT = HT[e % 2]
                for bk in range(NBK):
                    xr = xrows[nxr % 2]
                    nxr += 1
                    r0 = e * CAP + bk * 128
                    kb.ld("sp", V(xr), (S["XS"], S["XS"].t[r0:r0 + 128, :]))
                    for k in range(KD):
                        p = ptb[(k // 4) % 2]
                        kb.mm((p, p.t[:, (k % 4) * 128:(k % 4 + 1) * 128]), (xr, xr.t[:, k * 128:(k + 1) * 128]), V(identb), tr=True)
                        if k % 4 == 3 or k == KD - 1:
                            k0 = (k // 4) * 4
                            n = k - k0 + 1
                            kb.cp("act" if (k // 4) % 2 else "dve", (xT, xT.t[:, k0:k0 + n, bk * 128:(bk + 1) * 128]),
                                  (p, p.t[:, 0:n * 128].rearrange("p (a b) -> p a b", b=128)))
                wsrc = I["w_gu"].t[layer, e].rearrange("(k p) n -> p k n", p=128)
                for j in range(DEC):
                    wg = wgs[nwg % 3]
                    nwg += 1
                    kb.ld("pool", (wg, wg.t[:, :, 0, :]), (I["w_gu"], wsrc[:, :, j * 128:(j + 1) * 128]))
                    kb.ld("pool", (wg, wg.t[:, :, 1, :]), (I["w_gu"], wsrc[:, :, DE + j * 128:DE + (j + 1) * 128]), fill=True)
                    for hb in range(NHB):
                        c0 = hb * 512
                        n = min(512, CAP - c0)
                        pa = pab[npab % 4]
                        pb = pab[(npab + 1) % 4]
                        npab += 2
                        for k in range(KD):
                            kb.mm((pa, pa.t[:, 0:n]), (wg, wg.t[:, k, 0, :]), (xT, xT.t[:, k, c0:c0 + n]), start=(k == 0), stop=(k == KD - 1))
                        for k in range(KD):
                            kb.mm((pb, pb.t[:, 0:n]), (wg, wg.t[:, k, 1, :]), (xT, xT.t[:, k, c0:c0 + n]), start=(k == 0), stop=(k == KD - 1))
                        sa = sas[nsa % 2]
                        nsa += 1
                        kb.act((sa, sa.t[:, 0:n]), (pa, pa.t[:, 0:n]), AF.Silu)
                        kb.tt("dve", (hT, hT.t[:, j, c0:c0 + n]), (sa, sa.t[:, 0:n]), (pb, pb.t[:, 0:n]), ALU.mult)
                dsrc = I["w_dn"].t[layer, e].rearrange("(k p) n -> p k n", p=128)
                for nb in range(D // 512):
                    wd = wds[nwd % 3]
                    nwd += 1
                    kb.ld("pool", V(wd), (I["w_dn"], dsrc[:, :, nb * 512:(nb + 1) * 512]))
                    for bk in range(NBK):
                        py = pyy[npy % 2]
                        npy += 1
                        for j in range(DEC):
                            kb.mm(V(py), (hT, hT.t[:, j, bk * 128:(bk + 1) * 128]), (wd, wd.t[:, j, :]), start=(j == 0), stop=(j == DEC - 1))
                        yo = yos[nyo % 3]
                        nyo += 1
                        kb.cp("act" if nyo % 2 else "dve", V(yo), V(py))
                        r0 = e * CAP + bk * 128
                        kb.ld("sp", (S["YS"], S["YS"].t[r0:r0 + 128, nb * 512:(nb + 1) * 512]), V(yo), fill=True)
            WROW = max(2 * DE, D)
            wring = [kb.sb("wring%d" % i, [128, WROW], BF16) for i in range(3)]
            xTs = kb.sb("xTs", [128, KD, 128], BF16)
            Hs = kb.sb("Hs", [128, DE], BF16)
            sas_ = kb.sb("sas", [128, DE], F32)
            HTs = kb.sb("HTs", [128, DEC, 128], BF16)
            yfull = kb.sb("yfull", [128, D], F32)
            pcol = kb.sb("pcol", [128, 1], F32)
            kb.ld("sp", V(pcol), V(I["pcol"]))
            eio = kb.sb("eio", [128, NE], F32)
            kb.ts("dve", V(eio), V(ecap), 1.0 / CAP, None, op0=ALU.mult)
            t_a = kb.sb("t_a", [128, NE], F32)
            t_b = kb.sb("t_b", [128, NE], F32)
            scol = kb.sb("scol", [128, 4], F32)
            idxg = kb.sb("idxg", [128, NS, 2], I32)
            w_gu2d = I["w_gu"].t.rearrange("l e d n -> (l e d) n")
            w_dn2d = I["w_dn"].t.rearrange("l e d n -> (l e d) n")
            NGB = (2 * DE + 511) // 512
            NDB = D // 512
            nring = 0
            for b in range(NS):
                kb.ts("dve", V(t_a), V(BASE), float(b), None, op0=ALU.is_le)
                kb.tt("dve", V(t_b), V(BASE), V(NBLK), ALU.add)
                kb.ts("dve", V(t_b), V(t_b), float(b), None, op0=ALU.is_gt)
                kb.tt("dve", V(t_a), V(t_a), V(t_b), ALU.mult)
                kb.tt("dve", V(t_a), V(t_a), V(eio), ALU.mult)
                kb.red("dve", (scol, scol.t[:, 0:1]), V(t_a), ALU.add)
                kb.stt("dve", (scol, scol.t[:, 1:2]), (scol, scol.t[:, 0:1]), float(D), V(pcol), ALU.mult, ALU.add)
                kb.stt("dve", (scol, scol.t[:, 2:3]), (scol, scol.t[:, 0:1]), float(DE), V(pcol), ALU.mult, ALU.add)
                if layer > 0:
                    kb.ts("dve", (scol, scol.t[:, 1:2]), (scol, scol.t[:, 1:2]), float(layer * NE * D), None, op0=ALU.add)
                    kb.ts("dve", (scol, scol.t[:, 2:3]), (scol, scol.t[:, 2:3]), float(layer * NE * DE), None, op0=ALU.add)
                kb.cp("dve", (idxg, idxg.t[:, b, :]), (scol, scol.t[:, 1:3]))
                xr = xrows[nxr % 2]
                nxr += 1
                r0 = SP0 + b * 128
                kb.ld("sp", V(xr), (S["XS"], S["XS"].t[r0:r0 + 128, :]))
                for k in range(KD):
                    p = ptb[(k // 4) % 2]
                    kb.mm((p, p.t[:, (k % 4) * 128:(k % 4 + 1) * 128]), (xr, xr.t[:, k * 128:(k + 1) * 128]), V(identb), tr=True)
                    if k % 4 == 3 or k == KD - 1:
                        k0 = (k // 4) * 4
                        n = k - k0 + 1
                        kb.cp("act" if (k // 4) % 2 else "dve", (xTs, xTs.t[:, k0:k0 + n, :]),
                              (p, p.t[:, 0:n * 128].rearrange("p (a b) -> p a b", b=128)))
                for k in range(KD):
                    wk = wring[nring % 3]
                    nring += 1
                    kb.dma("pool", lambda h, wk=wk, b=b, k=k: h.indirect_dma_start(
                        out=wk.t[:, 0:2 * DE], out_offset=None, in_=w_gu2d,
                        in_offset=bass.IndirectOffsetOnAxis(ap=idxg.t[:, b, 0:1], axis=0), element_offset=k * 128 * 2 * DE),
                        reads=[I["w_gu"], idxg], writes=[wk])
                    for g4 in range(NGB):
                        n = min(512, 2 * DE - g4 * 512)
                        kb.mm((pab[g4], pab[g4].t[:, 0:n]), (xTs, xTs.t[:, k, :]), (wk, wk.t[:, g4 * 512:g4 * 512 + n]),
                              start=(k == 0), stop=(k == KD - 1))
                for c0 in range(0, DE, 512):
                    n = min(512, DE - c0)
                    ba, oa = c0 // 512, c0 % 512
                    bb_, ob = (DE + c0) // 512, (DE + c0) % 512
                    kb.act((sas_, sas_.t[:, c0:c0 + n]), (pab[ba], pab[ba].t[:, oa:oa + n]), AF.Silu)
                    kb.tt("dve", (Hs, Hs.t[:, c0:c0 + n]), (sas_, sas_.t[:, c0:c0 + n]), (pab[bb_], pab[bb_].t[:, ob:ob + n]), ALU.mult)
                for j in range(DEC):
                    p = ptb[(j // 4) % 2]
                    kb.mm((p, p.t[:, (j % 4) * 128:(j % 4 + 1) * 128]), (Hs, Hs.t[:, j * 128:(j + 1) * 128]), V(identb), tr=True)
                    if j % 4 == 3 or j == DEC - 1:
                        j0 = (j // 4) * 4
                        n = j - j0 + 1
                        kb.cp("act" if (j // 4) % 2 else "dve", (HTs, HTs.t[:, j0:j0 + n, :]),
                              (p, p.t[:, 0:n * 128].rearrange("p (a b) -> p a b", b=128)))
                for j in range(DEC):
                    wk = wring[nring % 3]
                    nring += 1
                    kb.dma("pool", lambda h, wk=wk, b=b, j=j: h.indirect_dma_start(
                        out=wk.t[:, 0:D], out_offset=None, in_=w_dn2d,
                        in_offset=bass.IndirectOffsetOnAxis(ap=idxg.t[:, b, 1:2], axis=0), element_offset=j * 128 * D),
                        reads=[I["w_dn"], idxg], writes=[wk])
                    for nb in range(NDB):
                        kb.mm(V(pab[nb]), (HTs, HTs.t[:, j, :]), (wk, wk.t[:, nb * 512:(nb + 1) * 512]), start=(j == 0), stop=(j == DEC - 1))
                for nb in range(NDB):
                    kb.cp("act" if nb % 2 else "dve", (yfull, yfull.t[:, nb * 512:(nb + 1) * 512]), V(pab[nb]))
                kb.ld("sp", (S["YS"], S["YS"].t[r0:r0 + 128, :]), V(yfull), fill=True)
            kb.barrier()
            kb.ctx = None

        with ExitStack() as sctx:
            kb.ctx = sctx
            gts = {0: kb.sb("GTc0", [128, D], F32)}
            M = S["MOD%d" % layer]
            self.load_bcast(kb, V(gts[0]), M.t[0, 5 * D:6 * D], M)
            if layer == 0:
                gts[1] = kb.sb("GTc1", [128, D], F32)
                self.load_bcast(kb, V(gts[1]), M.t[1, 5 * D:6 * D], M)
            if last:
                gfin = kb.sb("gfin", [128, D], F32)
                self.load_bcast(kb, V(gfin), I["gains"].t[4, :], I["gains"])
                ssq = kb.sb("ssq", [128, 1], F32)
                rstd = kb.sb("rstd", [128, 1], F32)
                junk = kb.sb("junk", [128, D], BF16)
            ygs = [[kb.sb("yg%d_%d" % (a, b), [128, D], F32) for b in range(2)] for a in range(2)]
            xts = [kb.sb("xt%d" % i, [128, D], F32) for i in range(2)]
            acc = [kb.sb("acc%d" % i, [128, D], F32) for i in range(2)]
            for ti, (cond, src, dst, i) in enumerate(tiles):
                xt = xts[ti % 2]
                kb.ld("sp", V(xt), (src, src.t[i * 128:(i + 1) * 128, :]))
                yg = ygs[ti % 2]
                for kk in range(2):
                    kb.memset("pool", V(yg[kk]), 0.0)
                    kb.dma("pool", lambda h, kk=kk, yg=yg, ti=ti: h.indirect_dma_start(
                        out=yg[kk].t[:, :], out_offset=None, in_=S["YS"].t[:, :],
                        in_offset=bass.IndirectOffsetOnAxis(ap=SL.t[:, ti, kk:kk + 1], axis=0),
                        bounds_check=breg, oob_is_err=False),
                        reads=[S["YS"], SL], writes=[yg[kk]])
                a = acc[ti % 2]
                kb.ts("dve", V(a), V(yg[0]), (WT, WT.t[:, ti, 0:1]), None, op0=ALU.mult)
                kb.stt("dve", V(a), V(yg[1]), (WT, WT.t[:, ti, 1:2]), V(a), ALU.mult, ALU.add)
                kb.tt("pool", V(a), V(a), V(gts[cond]), ALU.mult)
                kb.tt("dve", V(a), V(a), V(xt), ALU.add)
                if last:
                    kb.act(V(junk), V(a), AF.Square, accum=V(ssq))
                    self.rstd_of(kb, rstd, ssq, D)
                    kb.stt("dve", V(a), V(a), V(rstd), V(gfin), ALU.mult, ALU.mult)
                kb.ld("sp", (dst, dst.t[i * 128:(i + 1) * 128, :]), V(a), fill=True)
            kb.barrier()
            kb.ctx = None

    def phase_hgrn(self):
        kb, I, S = self.kb, self.I, self.S
        c = self.cfg
        D, KD, NT, NCT, L, LC = self.D, self.KD, self.NT, self.NCT, self.L, self.LC
        NH = c["NH"]
        SEG = self.SEG
        NSEG = L // SEG

        with ExitStack() as sctx:
            kb.ctx = sctx
            identb = kb.sb("identb", [128, 128], BF16)
            kb.ld("pool", V(identb), V(I["ident"]))
            ssq = kb.sb("ssq", [128, 1], F32)
            rstd = kb.sb("rstd", [128, 1], F32)
            junk = kb.sb("junk", [128, D], BF16)
            t1 = kb.sb("t1", [128, D], F32)
            xts = [kb.sb("xt%d" % i, [128, D], F32) for i in range(2)]
            hbs = [kb.sb("hb%d" % i, [128, D], BF16) for i in range(2)]
            hTg = [kb.sb("hTg%d" % i, [128, KD, 512], BF16) for i in range(2)]
            ptb = [kb.ps("ptb%d" % i, [128, 512], BF16) for i in range(2)]
            for (cond, src, dstT, ntile) in ((0, S["X2"], S["HXT"], NT), (1, S["C2"], S["HCT"], NCT)):
                with ExitStack() as s2:
                    kb.ctx = s2
                    G, SH, GT, gn = self.mod_vecs(kb, 1, cond, 0, 1, "h%d" % cond)
                    dview = dstT.t.rearrange("(k p) t -> p k t", p=128)
                    ng = 0
                    for i in range(ntile):
                        xt = xts[i % 2]
                        kb.ld("sp", V(xt), (src, src.t[i * 128:(i + 1) * 128, :]))
                        hb = hbs[i % 2]
                        self.rms_mod(kb, xt, G, SH, t1, hb, ssq, rstd, junk, D)
                        hg = hTg[ng % 2]
                        sub = i % 4
                        for k in range(KD):
                            p = ptb[(k // 4) % 2]
                            kb.mm((p, p.t[:, (k % 4) * 128:(k % 4 + 1) * 128]), (hb, hb.t[:, k * 128:(k + 1) * 128]), V(identb), tr=True)
                            if k % 4 == 3 or k == KD - 1:
                                k0 = (k // 4) * 4
                                n = k - k0 + 1
                                kb.cp("act" if (k // 4) % 2 else "dve", (hg, hg.t[:, k0:k0 + n, sub * 128:(sub + 1) * 128]),
                                      (p, p.t[:, 0:n * 128].rearrange("p (a b) -> p a b", b=128)))
                        if sub == 3 or i == ntile - 1:
                            t0 = (i // 4) * 512
                            n = (sub + 1) * 128
                            kb.ld("sp", (dstT, dview[:, :, t0:t0 + n]), (hg, hg.t[:, :, 0:n]), fill=True)
                            ng += 1
                    kb.barrier()
                    kb.ctx = sctx
            kb.barrier()
            kb.ctx = None

        with ExitStack() as sctx:
            kb.ctx = sctx
            NCS = SEG // 128
            identb = kb.sb("identb", [128, 128], BF16)
            kb.ld("pool", V(identb), V(I["ident"]))
            onesb = kb.sb("onesb", [128, 128], BF16)
            kb.memset("pool", V(onesb), 1.0)
            m128 = kb.sb("m128", [128, SEG], F32)
            kb.ld("sp", V(m128), V(I["mask128"]))
            m32 = kb.sb("m32", [128, SEG], F32)
            kb.ld("sp", V(m32), V(I["mask32"]))
            trim = kb.sb("trim", [128, 2, 128], F32)
            kb.ld("sp", V(trim), V(I["trimask"]))
            hn = kb.sb("hn", [128, 1], F32)
            kb.ld("sp", V(hn), V(I["hnorm"]))
            lbl = kb.sb("lbl", [128, 2, 2, NH], F32)
            kb.ld("sp", V(lbl), V(I["lbl"]))
            LB = kb.sb("LB", [128, 2, NH], F32)
            OML = kb.sb("OML", [128, 2, NH], F32)
            kb.tt("dve", V(LB), (lbl, lbl.t[:, 1]), (lbl, lbl.t[:, 0]), ALU.subtract)
            kb.act(V(LB), V(LB), AF.Sigmoid)
            kb.ts("dve", V(OML), V(LB), -1.0, 1.0, op0=ALU.mult, op1=ALU.add)
            NOML = kb.sb("NOML", [128, 2, NH], F32)
            kb.ts("dve", V(NOML), V(OML), -1.0, None, op0=ALU.mult)
            Sball = kb.sb("Sball", [128, SEG // 128, 128], BF16)
            wh = kb.sb("wh", [128, KD, 5, 128], BF16)
            HW = 256
            hts = [kb.sb("ht%d" % i, [128, KD, HW], BF16) for i in range(2)]
            hcT = kb.sb("hcT", [128, KD, LC], BF16)
            qT = kb.sb("qT", [128, L], BF16)
            vtok = kb.sb("vtok", [128, NT, 128], BF16)
            vctok = kb.sb("vctok", [128, NCT, 128], BF16)
            OT = kb.sb("OT", [128, L], F32)
            zTs = [kb.sb("zT%d" % i, [128, SEG], F32) for i in range(2)]
            lf = kb.sb("lf", [128, SEG], F32)
            kf = kb.sb("kf", [128, SEG], F32)
            bb = kb.sb("bb", [128, SEG], F32)
            bl = kb.sb("bl", [128, SEG], F32)
            cEb = kb.sb("cEb", [128, SEG], F32)
            clb = kb.sb("clb", [128, SEG], F32)
            rr = kb.sb("rr", [128, SEG], F32)
            e1 = kb.sb("e1", [128, SEG], F32)
            qe = kb.sb("qe", [128, SEG], BF16)
            qE = kb.sb("qE", [128, SEG], BF16)
            ke = [[kb.sb("ke%d_%d" % (d, i), [128, SEG], BF16) for i in range(4)] for d in range(2)]
            for d in range(2):
                for i in range(4):
                    kb.memset("pool", V(ke[d][i]), 0.0)
            kdT = kb.sb("kdT", [128, SEG], BF16)
            kdT_f = kb.sb("kdTf", [128, SEG], F32)
            kdtok = kb.sb("kdtok", [128, NCS, 128], BF16)
            At = kb.sb("At", [128, NCS, 128], BF16)
            sgTs = [kb.sb("sgT%d" % i, [128, SEG], BF16) for i in range(2)]
            rr2 = kb.sb("rr2", [128, SEG], F32)
            eb = kb.sb("eb", [128, NCS], F32)
            Sst = [kb.sb("S%d" % d, [128, 128], F32) for d in range(2)]
            Sbf = [kb.sb("Sb%d" % d, [128, 128], BF16) for d in range(2)]
            osum = kb.sb("osum", [128, 512], F32)
            osq = kb.sb("osq", [128, 512], BF16)
            rs = kb.sb("rs", [128, 512], F32)
            yo = [kb.sb("yo%d" % i, [128, 512], BF16) for i in range(2)]
            pz = [kb.ps("pz%d" % i, [128, 512]) for i in range(2)]
            pv = kb.ps("pv", [128, 512])
            pA = kb.ps("pA", [128, 512])
            ptk = kb.ps("ptk", [128, 512], BF16)
            po = kb.ps("po", [128, 512])
            pS = kb.ps("pS", [128, 512])
            pn = kb.ps("pn", [128, 512])
            cnt = {"pz": 0, "yo": 0, "ht": 0, "z": 0}
            hxv = S["HXT"].t.rearrange("(k p) t -> p k t", p=128)
            hcv = S["HCT"].t.rearrange("(k p) t -> p k t", p=128)
            ytv = S["YT"].t.rearrange("(h p) t -> p h t", p=128)

            fill = []

            def run_fill(n=1):
                for _ in range(n):
                    if fill:
                        fill.pop(0)()

            def v3(b, n, inner=128):
                return b.t[:, 0:n].rearrange("p (c s) -> p c s", s=inner)

            def proj_fm(hT, n, seg, evac):
                p = pz[cnt["pz"] % 2]
                cnt["pz"] += 1
                for k in range(KD):
                    kb.mm((p, p.t[:, 0:n]), (wh, wh.t[:, k, seg, :]), (hT, hT.t[:, k, 0:n]), start=(k == 0), stop=(k == KD - 1))
                evac((p, p.t[:, 0:n]))

            def proj_v(hT, n, dst, c0):
                nsub = n // 128
                for j in range(nsub):
                    for k in range(KD):
                        kb.mm((pv, pv.t[:, j * 128:(j + 1) * 128]), (hT, hT.t[:, k, j * 128:(j + 1) * 128]), (wh, wh.t[:, k, 4, :]),
                              start=(k == 0), stop=(k == KD - 1))
                kb.cp("act", (dst, dst.t[:, c0:c0 + nsub, :]), (pv, pv.t[:, 0:n].rearrange("p (a b) -> p a b", b=128)))

            def gla_seg(h, d, nch, zsrc, qsrc, vt, vc0, need_o, t0, sgT=None):
                n = nch * 128
                S_ = Sst[d]
                lbv = (LB, LB.t[:, d, h:h + 1])
                omv = (OML, OML.t[:, d, h:h + 1])
                nomv = (NOML, NOML.t[:, d, h:h + 1])
                if need_o:
                    if d == 0:
                        kb.act(qsrc, qsrc, AF.Silu)
                    else:
                        kb.act((sgT, sgT.t[:, 0:n]), (sgT, sgT.t[:, 0:n]), AF.Silu)
                kb.act((e1, e1.t[:, 0:n]), (zsrc, zsrc.t[:, 0:n]), AF.Sigmoid)
                kb.act((lf, lf.t[:, 0:n]), (e1, e1.t[:, 0:n]), AF.Ln, bias=lbv, scale=omv)
                kb.ts("pool", (kf, kf.t[:, 0:n]), (e1, e1.t[:, 0:n]), nomv, omv, op0=ALU.mult, op1=ALU.add)
                kb.op("dve", lambda hh: hh.tensor_tensor_scan(out=bb.t[:, 0:n], data0=m128.t[:, 0:n], data1=lf.t[:, 0:n],
                                                               initial=0.0, op0=ALU.mult, op1=ALU.add),
                      reads=[m128, lf], writes=[bb])
                if need_o:
                    kb.op("dve", lambda hh: hh.tensor_tensor_scan(out=bl.t[:, 0:n], data0=m32.t[:, 0:n], data1=lf.t[:, 0:n],
                                                                   initial=0.0, op0=ALU.mult, op1=ALU.add),
                          reads=[m32, lf], writes=[bl])
                if d == 0:
                    cE, cl = bb, bl
                    last = 127
                else:
                    cE, cl = cEb, clb
                    last = 0
                    kb.tt("dve", (rr, rr.t[:, 0:n]), (lf, lf.t[:, 0:n]), (bb, bb.t[:, 0:n]), ALU.subtract)
                    kb.tt("dve", (cEb, v3(cEb, n)), (rr, v3(rr, n)), (bb, v3(bb, n)[:, :, 127:128].to_broadcast([128, nch, 128])), ALU.add)
                    if need_o:
                        kb.tt("dve", (rr, rr.t[:, 0:n]), (lf, lf.t[:, 0:n]), (bl, bl.t[:, 0:n]), ALU.subtract)
                        kb.tt("dve", (clb, v3(clb, n, 32)), (rr, v3(rr, n, 32)),
                              (bl, v3(bl, n, 32)[:, :, 31:32].to_broadcast([128, nch * 4, 32])), ALU.add)
                cE3 = v3(cE, n)
                run_fill()
                if need_o:
                    kb.act((e1, e1.t[:, 0:n]), (cl, cl.t[:, 0:n]), AF.Exp)
                    kb.tt("pool", (qe, qe.t[:, 0:n]), (e1, e1.t[:, 0:n]), qsrc, ALU.mult)
                    run_fill()
                    kb.act((kdT_f, kdT_f.t[:, 0:n]), (cE, cE.t[:, 0:n]), AF.Exp)
                    kb.tt("pool", (qE, qE.t[:, 0:n]), (kdT_f, kdT_f.t[:, 0:n]), qsrc, ALU.mult)
                    for i4 in range(4):
                        if d == 0:
                            r0, r1 = 0, 32 * (i4 + 1)
                            ref = None if i4 == 0 else cE3[:, :, 32 * i4 - 1:32 * i4]
                        else:
                            r0, r1 = 32 * i4, 128
                            ref = None if i4 == 3 else cE3[:, :, 32 * (i4 + 1):32 * (i4 + 1) + 1]
                        nr = r1 - r0
                        rb = rr if i4 % 2 == 0 else rr2
                        rv = v3(rb, n)[:, :, r0:r1]
                        if ref is None:
                            kb.ts("dve", (rb, rv), (cE, cE3[:, :, r0:r1]), -1.0, None, op0=ALU.mult)
                        else:
                            kb.stt("dve", (rb, rv), (cE, cE3[:, :, r0:r1]), -1.0, (cE, ref.to_broadcast([128, nch, nr])), ALU.mult, ALU.add)
                        kb.act((rb, rv), (rb, rv), AF.Exp)
                        kb.tt("pool", (ke[d][i4], v3(ke[d][i4], n)[:, :, r0:r1]), (rb, rv), (kf, v3(kf, n)[:, :, r0:r1]), ALU.mult)
                        run_fill()
                    for c0 in range(0, nch, 4):
                        nc4 = min(4, nch - c0)
                        for cc in range(nc4):
                            ch = c0 + cc
                            for i4 in range(4):
                                kb.mm((pA, pA.t[:, cc * 128 + 32 * i4:cc * 128 + 32 * i4 + 32]),
                                      (ke[d][i4], ke[d][i4].t[:, ch * 128:(ch + 1) * 128]),
                                      (qe, qe.t[:, ch * 128 + 32 * i4:ch * 128 + 32 * i4 + 32]))
                        kb.tt("dve", (At, At.t[:, c0:c0 + nc4, :]), (pA, pA.t[:, 0:nc4 * 128].rearrange("p (a b) -> p a b", b=128)),
                              (trim, trim.t[:, d:d + 1, :].to_broadcast([128, nc4, 128])), ALU.mult)
                run_fill()
                kb.stt("dve", (rr, v3(rr, n)), (cE, cE3), -1.0, (cE, cE3[:, :, last:last + 1].to_broadcast([128, nch, 128])), ALU.mult, ALU.add)
                kb.act((rr, rr.t[:, 0:n]), (rr, rr.t[:, 0:n]), AF.Exp)
                kb.tt("pool", (kdT, kdT.t[:, 0:n]), (rr, rr.t[:, 0:n]), (kf, kf.t[:, 0:n]), ALU.mult)
                kb.act((eb, eb.t[:, 0:nch]), (cE, cE3[:, :, last]), AF.Exp)
                for c0 in range(0, nch, 4):
                    nc4 = min(4, nch - c0)
                    for cc in range(nc4):
                        ch = c0 + cc
                        kb.mm((ptk, ptk.t[:, cc * 128:(cc + 1) * 128]), (kdT, kdT.t[:, ch * 128:(ch + 1) * 128]), V(identb), tr=True)
                    kb.cp("act", (kdtok, kdtok.t[:, c0:c0 + nc4, :]), (ptk, ptk.t[:, 0:nc4 * 128].rearrange("p (a b) -> p a b", b=128)))
                run_fill()
                order = list(range(nch)) if d == 0 else list(range(nch - 1, -1, -1))
                for g0 in range(0, nch, 4):
                    grp = order[g0:g0 + 4]
                    for gi, ch in enumerate(grp):
                        kb.mm((pS, pS.t[:, gi * 128:(gi + 1) * 128]), (kdtok, kdtok.t[:, ch, :]), (vt, vt.t[:, vc0 + ch, :]))
                    for gi, ch in enumerate(grp):
                        if need_o:
                            kb.cp("dve", (Sball, Sball.t[:, ch, :]), V(S_))
                        kb.stt("dve", V(S_), V(S_), (eb, eb.t[:, ch:ch + 1]), (pS, pS.t[:, gi * 128:(gi + 1) * 128]), ALU.mult, ALU.add)
                run_fill()
                if need_o:
                    for g0 in range(0, nch, 4):
                        run_fill(2)
                        grp = order[g0:g0 + 4]
                        for ch in grp:
                            slot = ch % 4
                            pov = (po, po.t[:, slot * 128:(slot + 1) * 128])
                            kb.mm(pov, (vt, vt.t[:, vc0 + ch, :]), (At, At.t[:, ch, :]), start=True, stop=False)
                            kb.mm(pov, (Sball, Sball.t[:, ch, :]), (qE, qE.t[:, ch * 128:(ch + 1) * 128]), start=False, stop=True)
                        c_lo = min(grp)
                        ng_ = len(grp) * 128
                        tgg = t0 + c_lo * 128
                        if d == 0:
                            kb.cp("act", (OT, OT.t[:, tgg:tgg + ng_]), (po, po.t[:, 0:ng_]))
                        else:
                            kb.tt("dve", (osum, osum.t[:, 0:ng_]), (po, po.t[:, 0:ng_]), (OT, OT.t[:, tgg:tgg + ng_]), ALU.add)
                            kb.tt("pool", (osq, osq.t[:, 0:ng_]), (osum, osum.t[:, 0:ng_]), (osum, osum.t[:, 0:ng_]), ALU.mult)
                            kb.mm((pn, pn.t[:, 0:ng_]), V(onesb), (osq, osq.t[:, 0:ng_]))
                            kb.ts("dve", (rs, rs.t[:, 0:ng_]), (pn, pn.t[:, 0:ng_]), 1.0 / 128, EPS, op0=ALU.mult, op1=ALU.add)
                            kb.act((rs, rs.t[:, 0:ng_]), (rs, rs.t[:, 0:ng_]), AF.Ln)
                            kb.act((rs, rs.t[:, 0:ng_]), (rs, rs.t[:, 0:ng_]), AF.Exp, scale=-0.5)
                            kb.tt("pool", (rs, rs.t[:, 0:ng_]), (rs, rs.t[:, 0:ng_]), (osum, osum.t[:, 0:ng_]), ALU.mult)
                            y_ = yo[cnt["yo"] % 2]
                            cnt["yo"] += 1
                            kb.stt("dve", (y_, y_.t[:, 0:ng_]), (rs, rs.t[:, 0:ng_]), V(hn), (sgT, sgT.t[:, c_lo * 128:c_lo * 128 + ng_]), ALU.mult, ALU.mult)
                            kb.ld("sp", (S["YT"], ytv[:, h, tgg:tgg + ng_]), (y_, y_.t[:, 0:ng_]), fill=True)

            for h in range(NH):
                for sg in range(5):
                    kb.ld("pool", (wh, wh.t[:, :, sg, :]),
                          (I["w_in"], I["w_in"].t.rearrange("(k p) n -> p k n", p=128)[:, :, sg * D + h * 128:sg * D + (h + 1) * 128]),
                          fill=(sg > 0))
                kb.ld("sp", V(hcT), (S["HCT"], hcv))
                for d in range(2):
                    kb.memset("pool", V(Sst[d]), 0.0)
                proj_v(hcT, LC, vctok, 0)
                for d in range(2):
                    zb_ = zTs[cnt["z"] % 2]
                    cnt["z"] += 1
                    proj_fm(hcT, LC, 2 + d, lambda pv_, zb_=zb_: kb.cp("dve", (zb_, zb_.t[:, 0:LC]), pv_))
                    gla_seg(h, d, NCT, zb_, None, vctok, 0, False, 0)
                steps = [(0, sgi) for sgi in range(NSEG)] + [(1, sgi) for sgi in range(NSEG - 1, -1, -1)]
                cur = {}

                def make_units(d, sgi):
                    t0 = sgi * SEG
                    zT = zTs[cnt["z"] % 2]
                    sgT = sgTs[cnt["z"] % 2]
                    cnt["z"] += 1
                    us = []
                    for half in range(SEG // HW):
                        tt0 = t0 + half * HW

                        def u_first(tt0=tt0, half=half, zT=zT, sgT=sgT):
                            ht = hts[cnt["ht"] % 2]
                            cnt["ht"] += 1
                            cur["ht"] = ht
                            kb.ld("sp", V(ht), (S["HXT"], hxv[:, :, tt0:tt0 + HW]))
                            proj_fm(ht, HW, 2 + d, lambda pv_: kb.cp("dve", (zT, zT.t[:, half * HW:(half + 1) * HW]), pv_))

                        def u_second(tt0=tt0, half=half, sgT=sgT):
                            ht = cur["ht"]
                            if d == 0:
                                proj_fm(ht, HW, 0, lambda pv_: kb.cp("act", (qT, qT.t[:, tt0:tt0 + HW]), pv_))
                            else:
                                proj_fm(ht, HW, 1, lambda pv_: kb.cp("act", (sgT, sgT.t[:, half * HW:(half + 1) * HW]), pv_))

                        def u_third(tt0=tt0):
                            proj_v(cur["ht"], HW, vtok, tt0 // 128)
                        us += [u_first, u_second] + ([u_third] if d == 0 else [])
                    return us, zT, sgT

                nxt = make_units(*steps[0])
                for u in nxt[0]:
                    u()
                for si, (d, sgi) in enumerate(steps):
                    _, zT, sgT = nxt
                    if si + 1 < len(steps):
                        nxt = make_units(*steps[si + 1])
                        fill[:] = list(nxt[0])
                    t0 = sgi * SEG
                    gla_seg(h, d, NCS, zT, (qT, qT.t[:, t0:t0 + SEG]), vtok, t0 // 128, True, t0, sgT)
                    run_fill(len(fill))
            kb.barrier()
            kb.ctx = None

        with ExitStack() as sctx:
            kb.ctx = sctx
            wo = kb.sb("wo", [128, KD, D], BF16)
            kb.ld("pool", V(wo), (I["w_out"], I["w_out"].t.rearrange("(k p) n -> p k n", p=128)))
            GT = kb.sb("GTo", [128, D], F32)
            M = S["MOD1"]
            self.load_bcast(kb, V(GT), M.t[0, 2 * D:3 * D], M)
            yTs = [kb.sb("yT%d" % i, [128, NH, 512], BF16) for i in range(2)]
            xts = [kb.sb("xt%d" % i, [128, D], F32) for i in range(2)]
            ots = [kb.sb("ot%d" % i, [128, D], F32) for i in range(2)]
            pw = [kb.ps("pw%d" % i, [128, 512]) for i in range(4)]
            ytv = S["YT"].t.rearrange("(h p) t -> p h t", p=128)
            npw = 0
            for g in range((L + 511) // 512):
                t0 = g * 512
                n = min(512, L - t0)
                yT = yTs[g % 2]
                kb.ld("sp", (yT, yT.t[:, :, 0:n]), (S["YT"], ytv[:, :, t0:t0 + n]))
                for sub in range(n // 128):
                    i = (t0 // 128) + sub
                    xt = xts[i % 2]
                    kb.ld("sp", V(xt), (S["X2"], S["X2"].t[i * 128:(i + 1) * 128, :]))
                    ot = ots[i % 2]
                    for nb in range(D // 512):
                        p = pw[npw % 4]
                        npw += 1
                        for hh in range(NH):
                            kb.mm(V(p), (yT, yT.t[:, hh, sub * 128:(sub + 1) * 128]), (wo, wo.t[:, hh, nb * 512:(nb + 1) * 512]),
                                  start=(hh == 0), stop=(hh == NH - 1))
                        kb.tt("dve", (ot, ot.t[:, nb * 512:(nb + 1) * 512]), V(p), (GT, GT.t[:, nb * 512:(nb + 1) * 512]), ALU.mult)
                    kb.tt("pool", V(ot), V(ot), V(xt), ALU.add)
                    kb.ld("sp", (S["X3"], S["X3"].t[i * 128:(i + 1) * 128, :]), V(ot), fill=True)
            kb.barrier()
            kb.ctx = None


def make_in_maps(prog, inp):
    c = prog.cfg
    D, L, LC, KD, NE = prog.D, prog.L, prog.LC, prog.KD, prog.NE
    NH, CAP = c["NH"], c["CAP"]
    f = lambda a: np.ascontiguousarray(np.asarray(a, dtype=np.float32))
    gains = f(np.stack([inp["norm_mix"][0], inp["norm_mix"][1], inp["norm_ffn"][0], inp["norm_ffn"][1],
                        inp["norm_final"], inp["pool_scale"][0]], 0))
    wr = f(np.concatenate([inp["router_w_group"], inp["router_w_expert"]], -1))
    br = np.concatenate([inp["router_b_group"], inp["router_b_expert"]], -1)
    br = f(np.broadcast_to(br[:, None, :], (2, 128, br.shape[-1])))
    b_ada = f(np.broadcast_to(np.asarray(inp["b_ada"])[:, None, :], (2, 2, 6 * D)))
    ecap = f(np.broadcast_to((np.arange(NE) * CAP)[None, :], (128, NE)))
    tri = f(np.triu(np.ones((128, 128)), 1))
    lbl = f(np.asarray(inp["hgrn_lb_logits"]).reshape(2, 2, NH, 128).transpose(3, 0, 1, 2))
    hnorm = f(np.asarray(inp["hgrn_norm"]).reshape(128, 1))
    SEG = min(1024, L)
    m128 = np.ones((128, SEG), np.float32); m128[:, ::128] = 0
    m32 = np.ones((128, SEG), np.float32); m32[:, ::32] = 0
    st = np.arange(128)
    trimask = f(np.stack([(st[:, None] <= st[None, :]), (st[:, None] >= st[None, :])], 1))
    shared = dict(w_ada=f(inp["w_ada"]), b_ada=b_ada, gains=gains, pool_w=f(inp["pool_w"][0]),
                  bgrid=prog.Bgrid, rcgrid=prog.RCgrid, bseq=prog.Bseq, rcseq=prog.RCseq,
                  ident=np.eye(128, dtype=np.float32), wr=wr, br=br, ecap=ecap, tri=tri,
                  pcol=f(np.arange(128).reshape(128, 1)),
                  w_gu=f(inp["moe_w_gate_up"]), w_dn=f(inp["moe_w_down"]),
                  w_in=f(inp["hgrn_w_in"][0]), w_out=f(inp["hgrn_w_out"][0]), lbl=lbl, hnorm=hnorm,
                  mask128=m128, mask32=m32, trimask=trimask)
    maps = []
    for b in range(c["NCORES"]):
        cvec = np.stack([np.asarray(inp["c"][b]).reshape(KD, 128).T, np.asarray(inp["c_ctx"]).reshape(KD, 128).T], -1)
        m = dict(shared)
        m["x"] = f(inp["x"][b])
        m["ctx"] = f(inp["ctx"][b])
        m["cvec"] = f(cvec)
        maps.append(m)
    return maps


_CACHE = {}


def run(inp, cfg, debug=False, stop_after=None):
    prog = Prog(cfg, debug=debug, stop_after=stop_after)
    nc = prog.build()
    maps = make_in_maps(prog, inp)
    res = run_bass_kernel_spmd(nc, maps, core_ids=list(range(cfg["NCORES"])))
    return prog, res


def kernel(**inputs):
    cfg = FULL_CFG
    prog, res = run(inputs, cfg)
    out = np.stack([np.asarray(r["out"]) for r in res.results], 0)
    return out.astype(np.float32)
```
